# Optimizing a Trainium2 kernel written in Bass

```python
import jax, jax.numpy as jnp
from jax import lax
import numpy as np

D_MODEL = 2048
BATCH = 4
SEQ = 2048
DEPTH = 2

HEAD_DIM = 64
FOX_HEADS = D_MODEL // (4 * HEAD_DIM)
DIL_HEADS = D_MODEL // (4 * HEAD_DIM)
MLSTM_HEADS = 4
MLSTM_DV = D_MODEL // (2 * MLSTM_HEADS)
MLSTM_DQK = MLSTM_DV // 2
FOX_W = FOX_HEADS * HEAD_DIM
DIL_W = DIL_HEADS * HEAD_DIM
MLSTM_QKW = MLSTM_HEADS * MLSTM_DQK
MLSTM_W = MLSTM_HEADS * MLSTM_DV
D_MIX = FOX_W + DIL_W + MLSTM_W
D_IN = 3 * FOX_W + 3 * DIL_W + 2 * MLSTM_QKW + 2 * MLSTM_W + FOX_HEADS + 2 * MLSTM_HEADS
CONV_WIDTH = 4
MLSTM_CHUNK = 64
Q_BLOCK = 128
DIL_CONFIGS = ((128, 1), (512, 4), (2048, 16))
ROPE_THETA = 10000.0
PEER_HEADS = 8
PEER_NKEYS = 128
PEER_EXPERTS = PEER_NKEYS * PEER_NKEYS
PEER_DKEY = 256
PEER_TOPK = 16
PEER_TOKEN_BLOCK = 128
EPS = 1e-6

kernel_name = 'hybrid_fox_mlstm_dilated_peer'


def _split_points():
    sizes = (FOX_W, FOX_W, FOX_W, DIL_W, DIL_W, DIL_W, MLSTM_QKW, MLSTM_QKW,
             MLSTM_W, MLSTM_W, FOX_HEADS, MLSTM_HEADS, MLSTM_HEADS)
    return [int(p) for p in np.cumsum(sizes)[:-1]]


def _rmsnorm(x, g=None):
    xf = x.astype(jnp.float32)
    y = xf * lax.rsqrt(jnp.mean(xf * xf, axis=-1, keepdims=True) + EPS)
    if g is not None:
        y = y * g.astype(jnp.float32)
    return y.astype(x.dtype)


def _heads(t, n_heads):
    B, S, _ = t.shape
    return t.reshape(B, S, n_heads, -1).transpose(0, 2, 1, 3)


def _rope(t):
    S, hd = t.shape[2], t.shape[3]
    inv = jnp.power(ROPE_THETA, -jnp.arange(0, hd, 2, dtype=jnp.float32) / hd)
    ang = jnp.arange(S, dtype=jnp.float32)[:, None] * inv[None, :]
    cos, sin = jnp.cos(ang), jnp.sin(ang)
    tf = t.astype(jnp.float32)
    t1, t2 = tf[..., : hd // 2], tf[..., hd // 2:]
    return jnp.concatenate([t1 * cos - t2 * sin, t2 * cos + t1 * sin], axis=-1).astype(t.dtype)


def _causal_conv(t, w):
    S = t.shape[1]
    K = w.shape[0]
    tp = jnp.pad(t, ((0, 0), (K - 1, 0), (0, 0)))
    out = tp[:, 0:S] * w[0]
    for j in range(1, K):
        out = out + tp[:, j:j + S] * w[j]
    return out


def _fox_attention(q, k, v, logf):
    B, H, S, hd = q.shape
    nb = S // Q_BLOCK
    F = jnp.cumsum(logf.astype(jnp.float32), axis=-1)
    qb = jnp.moveaxis(q.reshape(B, H, nb, Q_BLOCK, hd), 2, 0)
    Fb = jnp.moveaxis(F.reshape(B, H, nb, Q_BLOCK), 2, 0)
    kpos = jnp.arange(S)

    def block(args):
        qi, Fi, i = args
        s = jnp.einsum('bhqd,bhkd->bhqk', qi, k).astype(jnp.float32)
        s = s + Fi[..., :, None] - F[..., None, :]
        qpos = i * Q_BLOCK + jnp.arange(Q_BLOCK)
        s = jnp.where(kpos[None, :] <= qpos[:, None], s, -jnp.inf)
        p = jax.nn.softmax(s, axis=-1)
        return jnp.einsum('bhqk,bhkd->bhqd', p.astype(v.dtype), v)

    o = lax.map(block, (qb, Fb, jnp.arange(nb)))
    return jnp.moveaxis(o, 0, 2).reshape(B, H, S, hd)


def _banded_stats(q, k, v, span):
    B, H, G, L, hd = q.shape
    nb = -(-L // Q_BLOCK)
    Lp = nb * Q_BLOCK
    qb = jnp.pad(q, ((0, 0), (0, 0), (0, 0), (0, Lp - L), (0, 0))).reshape(B, H, G, nb, Q_BLOCK, hd)

    def two_blocks(t):
        tb = jnp.pad(t, ((0, 0), (0, 0), (0, 0), (Q_BLOCK, Lp - L), (0, 0)))
        tb = tb.reshape(B, H, G, nb + 1, Q_BLOCK, hd)
        return jnp.concatenate([tb[:, :, :, :-1], tb[:, :, :, 1:]], axis=4)

    kb, vb = two_blocks(k), two_blocks(v)
    s = jnp.einsum('bhgnqd,bhgnkd->bhgnqk', qb, kb).astype(jnp.float32)
    start = jnp.arange(nb)[:, None, None] * Q_BLOCK
    qpos = start + jnp.arange(Q_BLOCK)[None, :, None]
    kpos = start - Q_BLOCK + jnp.arange(2 * Q_BLOCK)[None, None, :]
    dist = qpos - kpos
    mask = (dist >= 0) & (dist <= span) & (kpos >= 0)
    s = jnp.where(mask, s, -jnp.inf)
    m = jnp.max(s, axis=-1)
    p = jnp.exp(s - m[..., None])
    l = jnp.sum(p, axis=-1)
    acc = jnp.einsum('bhgnqk,bhgnkd->bhgnqd', p, vb.astype(jnp.float32))
    return (m.reshape(B, H, G, Lp)[..., :L],
            l.reshape(B, H, G, Lp)[..., :L],
            acc.reshape(B, H, G, Lp, hd)[..., :L, :])


def _dilated_attention(q, k, v):
    B, H, S, hd = q.shape
    ms, ls, accs = [], [], []
    for window, dil in DIL_CONFIGS:
        L = S // dil

        def to_res(t, dil=dil, L=L):
            return t.reshape(B, H, L, dil, hd).transpose(0, 1, 3, 2, 4)

        m, l, acc = _banded_stats(to_res(q), to_res(k), to_res(v), window // dil)
        ms.append(m.transpose(0, 1, 3, 2).reshape(B, H, S))
        ls.append(l.transpose(0, 1, 3, 2).reshape(B, H, S))
        accs.append(acc.transpose(0, 1, 3, 2, 4).reshape(B, H, S, hd))
    m_all = jnp.stack(ms)
    w = jnp.exp(m_all - jnp.max(m_all, axis=0, keepdims=True))
    den = jnp.sum(w * jnp.stack(ls), axis=0)
    num = jnp.sum(w[..., None] * jnp.stack(accs), axis=0)
    return (num / den[..., None]).astype(v.dtype)


def _mlstm(q, k, v, i_pre, f_pre):
    out_dtype = v.dtype
    B, H, S, dqk = q.shape
    dv = v.shape[-1]
    L = MLSTM_CHUNK
    nc = S // L
    f32 = jnp.float32
    q = q.astype(f32)
    k = k.astype(f32) * (dqk ** -0.5)
    v = v.astype(f32)
    i_pre = i_pre.astype(f32)
    logf = jax.nn.log_sigmoid(f_pre.astype(f32))

    def chunks(t):
        return jnp.moveaxis(t.reshape((B, H, nc, L) + t.shape[3:]), 2, 0)

    tri = jnp.tril(jnp.ones((L, L), dtype=bool))

    def step(carry, xs):
        C, n, m = carry
        qc, kc, vc, ic, fc = xs
        b = jnp.cumsum(fc, axis=-1)
        D = jnp.where(tri, b[..., :, None] - b[..., None, :] + ic[..., None, :], -jnp.inf)
        g = b + m[..., None]
        mt = jnp.maximum(g, jnp.max(D, axis=-1))
        qk_w = jnp.einsum('bhtk,bhsk->bhts', qc, kc) * jnp.exp(D - mt[..., None])
        inter = jnp.exp(g - mt)
        num = (jnp.einsum('bhts,bhsv->bhtv', qk_w, vc)
               + inter[..., None] * jnp.einsum('bhtk,bhvk->bhtv', qc, C))
        den = jnp.sum(qk_w, axis=-1) + inter * jnp.einsum('bhtk,bhk->bht', qc, n)
        h = num / jnp.maximum(jnp.abs(den), jnp.exp(-mt))[..., None]
        bL = b[..., -1]
        dec = bL[..., None] - b + ic
        m_new = jnp.maximum(bL + m, jnp.max(dec, axis=-1))
        w = jnp.exp(dec - m_new[..., None])
        keep = jnp.exp(bL + m - m_new)
        C_new = keep[..., None, None] * C + jnp.einsum('bhs,bhsv,bhsk->bhvk', w, vc, kc)
        n_new = keep[..., None] * n + jnp.einsum('bhs,bhsk->bhk', w, kc)
        return (C_new, n_new, m_new), h

    init = (jnp.zeros((B, H, dv, dqk), f32), jnp.zeros((B, H, dqk), f32), jnp.zeros((B, H), f32))
    _, h = lax.scan(step, init, (chunks(q), chunks(k), chunks(v), chunks(i_pre), chunks(logf)))
    return jnp.moveaxis(h, 0, 2).reshape(B, H, S, dv).astype(out_dtype)


def _hybrid_mixer(h, w_in, fox_fb, mlstm_ib, mlstm_fb, fox_qn, fox_kn, dil_qn, dil_kn,
                  mlstm_conv, out_norm, w_out):
    B, S, _ = h.shape
    proj = h @ w_in
    (fq, fk, fv, dq, dk, dv, mq, mk, mv, mo, ff, mi, mf) = jnp.split(proj, _split_points(), axis=-1)
    scale = HEAD_DIM ** -0.5
    fq = _rmsnorm(_heads(fq, FOX_HEADS), fox_qn) * scale
    fk = _rmsnorm(_heads(fk, FOX_HEADS), fox_kn)
    logf = jax.nn.log_sigmoid((ff + fox_fb).astype(jnp.float32)).transpose(0, 2, 1)
    o_fox = _fox_attention(fq, fk, _heads(fv, FOX_HEADS), logf)
    dq = _rope(_rmsnorm(_heads(dq, DIL_HEADS), dil_qn)) * scale
    dk = _rope(_rmsnorm(_heads(dk, DIL_HEADS), dil_kn))
    o_dil = _dilated_attention(dq, dk, _heads(dv, DIL_HEADS))
    qk = jax.nn.silu(_causal_conv(jnp.concatenate([mq, mk], axis=-1), mlstm_conv))
    mq, mk = jnp.split(qk, 2, axis=-1)
    h_m = _mlstm(_heads(mq, MLSTM_HEADS), _heads(mk, MLSTM_HEADS), _heads(mv, MLSTM_HEADS),
                 (mi + mlstm_ib).transpose(0, 2, 1), (mf + mlstm_fb).transpose(0, 2, 1))
    o_a = _rmsnorm(o_fox.transpose(0, 2, 1, 3)).reshape(B, S, FOX_W)
    o_b = _rmsnorm(o_dil.transpose(0, 2, 1, 3)).reshape(B, S, DIL_W)
    o_c = _rmsnorm(h_m.transpose(0, 2, 1, 3)).reshape(B, S, MLSTM_W) * jax.nn.sigmoid(mo)
    merged = jnp.concatenate([o_a, o_b, o_c], axis=-1) * out_norm
    return merged @ w_out


def _peer(y, wq, k1, k2, u_tab, v_tab):
    B, S, D = y.shape
    T = B * S
    yf = y.reshape(T, D)
    half = PEER_DKEY // 2
    q = (yf @ wq).reshape(T, PEER_HEADS, PEER_DKEY).astype(jnp.float32)
    s1 = jnp.einsum('thd,nd->thn', q[..., :half], k1.astype(jnp.float32))
    s2 = jnp.einsum('thd,nd->thn', q[..., half:], k2.astype(jnp.float32))
    v1, i1 = lax.top_k(s1, PEER_TOPK)
    v2, i2 = lax.top_k(s2, PEER_TOPK)
    cand = (v1[..., :, None] + v2[..., None, :]).reshape(T, PEER_HEADS, PEER_TOPK * PEER_TOPK)
    top_v, top_c = lax.top_k(cand, PEER_TOPK)
    e1 = jnp.take_along_axis(i1, top_c // PEER_TOPK, axis=-1)
    e2 = jnp.take_along_axis(i2, top_c % PEER_TOPK, axis=-1)
    experts = e1 * PEER_NKEYS + e2
    gates = jax.nn.softmax(top_v, axis=-1)
    nblk = T // PEER_TOKEN_BLOCK

    def block(args):
        xb, eb, gb = args
        a = jnp.einsum('td,thkd->thk', xb, u_tab[eb]).astype(jnp.float32)
        w = (gb * jax.nn.gelu(a)).astype(v_tab.dtype)
        return jnp.einsum('thk,thkd->td', w, v_tab[eb])

    out = lax.map(block, (yf.reshape(nblk, PEER_TOKEN_BLOCK, D),
                          experts.reshape(nblk, PEER_TOKEN_BLOCK, PEER_HEADS, PEER_TOPK),
                          gates.reshape(nblk, PEER_TOKEN_BLOCK, PEER_HEADS, PEER_TOPK)))
    return out.reshape(B, S, D).astype(y.dtype)


def setup_inputs(seed: int = 0) -> dict:
    key = jax.random.key(seed)
    ks = jax.random.split(key, 22)

    def nrm(k, shape, scale):
        return jax.random.normal(k, shape, jnp.float32) * scale

    def gain(k, shape):
        return 1.0 + 0.02 * jax.random.normal(k, shape, jnp.float32)

    return {
        'x': nrm(ks[0], (BATCH, SEQ, D_MODEL), 1.0),
        'c': nrm(ks[1], (BATCH, D_MODEL), 1.0),
        'ada_w': nrm(ks[2], (DEPTH, D_MODEL, 6 * D_MODEL), 0.5 * D_MODEL ** -0.5),
        'ada_b': nrm(ks[3], (DEPTH, 6 * D_MODEL), 0.02),
        'norm_mix': gain(ks[4], (DEPTH, D_MODEL)),
        'w_in': nrm(ks[5], (DEPTH, D_MODEL, D_IN), D_MODEL ** -0.5),
        'fox_fb': 2.0 + nrm(ks[6], (DEPTH, FOX_HEADS), 0.5),
        'mlstm_ib': nrm(ks[7], (DEPTH, MLSTM_HEADS), 0.1),
        'mlstm_fb': 3.0 + nrm(ks[8], (DEPTH, MLSTM_HEADS), 0.5),
        'fox_qn': gain(ks[9], (DEPTH, HEAD_DIM)),
        'fox_kn': gain(ks[10], (DEPTH, HEAD_DIM)),
        'dil_qn': gain(ks[11], (DEPTH, HEAD_DIM)),
        'dil_kn': gain(ks[12], (DEPTH, HEAD_DIM)),
        'mlstm_conv': nrm(ks[13], (DEPTH, CONV_WIDTH, 2 * MLSTM_QKW), CONV_WIDTH ** -0.5),
        'out_norm': gain(ks[14], (DEPTH, D_MIX)),
        'w_out': nrm(ks[15], (DEPTH, D_MIX, D_MODEL), D_MIX ** -0.5),
        'norm_ffn': gain(ks[16], (DEPTH, D_MODEL)),
        'peer_wq': nrm(ks[17], (DEPTH, D_MODEL, PEER_HEADS * PEER_DKEY), D_MODEL ** -0.5),
        'peer_k1': nrm(ks[18], (DEPTH, PEER_NKEYS, PEER_DKEY // 2), (PEER_DKEY // 2) ** -0.5),
        'peer_k2': nrm(ks[19], (DEPTH, PEER_NKEYS, PEER_DKEY // 2), (PEER_DKEY // 2) ** -0.5),
        'peer_u': nrm(ks[20], (DEPTH, PEER_EXPERTS, D_MODEL), D_MODEL ** -0.5),
        'peer_v': nrm(ks[21], (DEPTH, PEER_EXPERTS, D_MODEL), PEER_HEADS ** -0.5),
    }


def reference(x, c, ada_w, ada_b, norm_mix, w_in, fox_fb, mlstm_ib, mlstm_fb, fox_qn, fox_kn,
              dil_qn, dil_kn, mlstm_conv, out_norm, w_out, norm_ffn, peer_wq, peer_k1, peer_k2,
              peer_u, peer_v):
    cond = jax.nn.silu(c)
    for l in range(DEPTH):
        mod = cond @ ada_w[l] + ada_b[l]
        sh1, sc1, g1, sh2, sc2, g2 = [t[:, None, :] for t in jnp.split(mod, 6, axis=-1)]
        h = _rmsnorm(x, norm_mix[l]) * (1 + sc1) + sh1
        mix = _hybrid_mixer(h, w_in[l], fox_fb[l], mlstm_ib[l], mlstm_fb[l], fox_qn[l], fox_kn[l],
                            dil_qn[l], dil_kn[l], mlstm_conv[l], out_norm[l], w_out[l])
        x = x + g1 * mix
        y = _rmsnorm(x, norm_ffn[l]) * (1 + sc2) + sh2
        x = x + g2 * _peer(y, peer_wq[l], peer_k1[l], peer_k2[l], peer_u[l], peer_v[l])
    return x
```

```python
from contextlib import ExitStack
import numpy as np
import concourse.bass as bass
import concourse.mybir as mybir
from concourse.bass_utils import run_bass_kernel_spmd

F32 = mybir.dt.float32
BF16 = mybir.dt.bfloat16
I32 = mybir.dt.int32
U32 = mybir.dt.uint32
AF = mybir.ActivationFunctionType
ALU = mybir.AluOpType
AX = mybir.AxisListType

D = 2048
DIN = 6160
S = 2048
NT = 1024
EPS = 1e-6


class T:
    __slots__ = ("h", "w", "r", "name")

    def __init__(self, h, name=""):
        self.h = h
        self.w = {}
        self.r = {}
        self.name = name

    def __getitem__(self, idx):
        return self.h[idx]


class Eng:
    def __init__(self, fw, name, obj, nsem=1, inc=1, is_dma=None):
        self.name = name
        self.obj = obj
        self.inc = inc
        self.sems = [fw.nc.alloc_semaphore(f"s_{name}_{i}") for i in range(nsem)]
        self.cnt = [0] * nsem
        self.n = 0
        self.seen = {}
        self.is_dma = (inc == 16) if is_dma is None else is_dma


class FW:
    def __init__(self, nc, ndma_sems=8):
        self.nc = nc
        self.semtab = {}
        self.pe = Eng(self, "pe", nc.tensor)
        self.act = Eng(self, "act", nc.scalar)
        self.dve = Eng(self, "dve", nc.vector)
        self.pool = Eng(self, "pool", nc.gpsimd)
        self.sp = Eng(self, "sp", nc.sync, nsem=ndma_sems, inc=16)
        self.gq = Eng(self, "gq", nc.gpsimd, nsem=ndma_sems, inc=16)
        self.aq = Eng(self, "aq", nc.scalar, nsem=ndma_sems, inc=16)
        self.cc = Eng(self, "cc", nc.gpsimd, nsem=1, inc=1, is_dma=True)
        self.engs = (self.pe, self.act, self.dve, self.pool, self.sp, self.gq, self.aq, self.cc)
        self.stream = {"pe": self.pe, "act": self.act, "dve": self.dve, "pool": self.pool,
                       "sp": self.sp, "gq": self.pool, "aq": self.act, "cc": self.pool}
        for e in self.engs:
            for i, s in enumerate(e.sems):
                self.semtab[(e.name, i)] = s
        self.nwaits = 0
        self.ninst = 0
        self.es = None

    def sb(self, name, shape, dt):
        self.uid = getattr(self, "uid", 0) + 1
        name = f"{name}_{self.uid}"
        if self.es is not None:
            return T(self.es.enter_context(self.nc.sbuf_tensor(name, list(shape), dt)), name)
        return T(self.nc.alloc_sbuf_tensor(name, list(shape), dt), name)

    def ps(self, name, shape, dt=F32):
        self.uid = getattr(self, "uid", 0) + 1
        name = f"{name}_{self.uid}"
        if self.es is not None:
            return T(self.es.enter_context(self.nc.psum_tensor(name, list(shape), dt)), name)
        return T(self.nc.alloc_psum_tensor(name, list(shape), dt), name)

    def dram(self, name, shape, dt, kind="Internal"):
        return T(self.nc.dram_tensor(name, list(shape), dt, kind=kind), name)

    def _wait(self, eng, dep):
        if dep is None:
            return
        key, val = dep
        st = self.stream[eng.name]
        if st.seen.get(key, 0) >= val:
            return
        st.seen[key] = val
        st.obj.wait_ge(self.semtab[key], val)
        self.nwaits += 1

    def issue(self, eng, fn, reads=(), writes=()):
        if eng.is_dma:
            slot = eng.n % len(eng.sems)
            if eng.cnt[slot] > 0:
                self._wait(eng, ((eng.name, slot), eng.cnt[slot]))
        else:
            slot = 0
        mine = eng.name
        for t in reads:
            for k, v in t.w.items():
                if k[0] == mine and eng is self.pe:
                    continue
                self._wait(eng, (k, v))
        for t in writes:
            for k, v in t.w.items():
                if k[0] == mine and not eng.is_dma:
                    continue
                self._wait(eng, (k, v))
            for k, v in t.r.items():
                if k[0] == mine and not eng.is_dma:
                    continue
                self._wait(eng, (k, v))
        ins = fn()
        eng.n += 1
        eng.cnt[slot] += eng.inc
        ins.then_inc(eng.sems[slot], eng.inc)
        key = (eng.name, slot)
        val = eng.cnt[slot]
        for t in reads:
            if t.r.get(key, 0) < val:
                t.r[key] = val
        for t in writes:
            t.w[key] = val
            t.r.clear()
        self.ninst += 1
        return ins

    def barrier(self):
        for sname in ("pe", "act", "dve", "pool", "sp"):
            st = self.stream[sname]
            for e in self.engs:
                for i in range(len(e.sems)):
                    if e.cnt[i] > 0:
                        self._wait(st, ((e.name, i), e.cnt[i]))

    def dma(self, q, out_t, out_ap, in_t, in_ap, **kw):
        return self.issue(q, lambda: q.obj.dma_start(out=out_ap, in_=in_ap, **kw), reads=[in_t], writes=[out_t])


def alias(t, new_shape):
    v = T(t.h.reshape(list(new_shape)), t.name + "_v")
    v.w = t.w
    v.r = t.r
    return v


def make_identity(fw, name, dt):
    nc = fw.nc
    idn = fw.sb(name, [128, 128], dt)
    fw.issue(fw.pool, lambda: nc.gpsimd.memset(idn[:], 1.0), writes=[idn])
    fw.issue(fw.pool, lambda: nc.gpsimd.affine_select(out=idn[:], in_=idn[:], pattern=[[-1, 128]],
                                                      compare_op=ALU.is_equal, fill=0.0, base=0,
                                                      channel_multiplier=1), reads=[idn], writes=[idn])
    return idn


def bcast_row(ap_row, nparts=128):
    return ap_row.partition_broadcast(nparts)


def rstd_from_ss(fw, s_, inv_n, c0=0, c1=1, w=1):
    nc = fw.nc
    fw.issue(fw.dve, lambda: nc.vector.tensor_scalar(out=s_[:, c1:c1 + w], in0=s_[:, c0:c0 + w], scalar1=inv_n, scalar2=EPS,
                                                     op0=ALU.mult, op1=ALU.add), reads=[s_], writes=[s_])
    fw.issue(fw.act, lambda: nc.scalar.activation(out=s_[:, c1:c1 + w], in_=s_[:, c1:c1 + w], func=AF.Ln), reads=[s_], writes=[s_])
    fw.issue(fw.act, lambda: nc.scalar.activation(out=s_[:, c1:c1 + w], in_=s_[:, c1:c1 + w], func=AF.Exp, scale=-0.5), reads=[s_], writes=[s_])

def emit_mod(fw, cT, ada_w, ada_b, modd):
    nc = fw.nc
    with ExitStack() as es:
        fw.es = es
        cs = fw.sb("mod_c", [128, 16], F32)
        cb = fw.sb("mod_cb", [128, 16], BF16)
        wts = [fw.sb(f"mod_w{i}", [128, 16, 512], BF16) for i in range(2)]
        bts = [fw.sb(f"mod_b{i}", [1, 512], F32) for i in range(2)]
        sts = [fw.sb(f"mod_s{i}", [1, 512], F32) for i in range(2)]
        pss = [fw.ps(f"mod_p{i}", [128, 512], F32) for i in range(2)]
        fw.dma(fw.sp, cs, cs[:], cT, cT[:])
        fw.issue(fw.act, lambda: nc.scalar.activation(out=cb[:], in_=cs[:], func=AF.Silu), reads=[cs], writes=[cb])
        wv = ada_w.h.rearrange("(k p) j -> p k j", p=128)
        bv = ada_b.h.rearrange("(o j) -> o j", o=1)
        mv = modd.h.rearrange("(o j) -> o j", o=1)
        for jc in range(12):
            w = wts[jc % 2]; b = bts[jc % 2]; st = sts[jc % 2]; p = pss[jc % 2]
            js = slice(jc * 512, (jc + 1) * 512)
            fw.dma(fw.gq, w, w[:], ada_w, wv[:, :, js])
            fw.dma(fw.sp, b, b[:], ada_b, bv[:, js])
            for k in range(16):
                fw.issue(fw.pe, lambda k=k: nc.tensor.matmul(p[0:1, :], lhsT=cb[:, k:k + 1], rhs=w[:, k, :],
                                                             start=(k == 0), stop=(k == 15)),
                         reads=[cb, w], writes=[p])
            fw.issue(fw.dve, lambda: nc.vector.tensor_tensor(out=st[:], in0=p[0:1, :], in1=b[:], op=ALU.add),
                     reads=[p, b], writes=[st])
            fw.dma(fw.sp, modd, mv[:, js], st, st[:])
        fw.barrier()
        fw.es = None


def emit_norm_mod(fw, xsrc, modd, off_shift, off_scale, gain, hT, idb, nt_tiles=8, ydst=None):
    nc = fw.nc
    es = fw.es
    nm_b = fw.sb("nm_b", [128, D], F32)
    sc_b = fw.sb("sc_b", [128, D], F32)
    sh_b = fw.sb("sh_b", [128, D], F32)
    xts = [fw.sb(f"nm_x{i}", [128, D], F32) for i in range(2)]
    hf = fw.sb("nm_hf", [128, D], F32)
    hbs = [fw.sb(f"nm_hb{i}", [128, D], BF16) for i in range(2)]
    junk = fw.sb("nm_junk", [128, D], BF16)
    ss = [fw.sb(f"nm_ss{i}", [128, 2], F32) for i in range(2)]
    trs = [fw.ps(f"nm_tr{i}", [128, 1024], BF16) for i in range(2)]
    mv = modd.h.rearrange("(o j) -> o j", o=1)
    gv = gain.h.rearrange("(o j) -> o j", o=1)
    fw.dma(fw.sp, nm_b, nm_b[:], gain, gv[:, :].partition_broadcast(128))
    fw.dma(fw.sp, sc_b, sc_b[:], modd, mv[:, off_scale:off_scale + D].partition_broadcast(128))
    fw.dma(fw.sp, sh_b, sh_b[:], modd, mv[:, off_shift:off_shift + D].partition_broadcast(128))
    fw.issue(fw.dve, lambda: nc.vector.scalar_tensor_tensor(out=sc_b[:], in0=sc_b[:], scalar=1.0, in1=nm_b[:],
                                                            op0=ALU.add, op1=ALU.mult),
             reads=[sc_b, nm_b], writes=[sc_b])
    for i in range(nt_tiles):
        xt = xts[i % 2]; hb = hbs[i % 2]; s_ = ss[i % 2]
        fw.dma(fw.sp, xt, xt[:], xsrc, xsrc[i * 128:(i + 1) * 128, :])
        fw.issue(fw.act, lambda: nc.scalar.activation(out=junk[:], in_=xt[:], func=AF.Square, accum_out=s_[:, 0:1]),
                 reads=[xt], writes=[junk, s_])
        rstd_from_ss(fw, s_, 1.0 / D)
        fw.issue(fw.dve, lambda: nc.vector.scalar_tensor_tensor(out=hf[:], in0=xt[:], scalar=s_[:, 1:2], in1=sc_b[:],
                                                                op0=ALU.mult, op1=ALU.mult),
                 reads=[xt, s_, sc_b], writes=[hf])
        fw.issue(fw.pool, lambda: nc.gpsimd.tensor_tensor(out=hb[:], in0=hf[:], in1=sh_b[:], op=ALU.add),
                 reads=[hf, sh_b], writes=[hb])
        if ydst is not None:
            fw.dma(fw.aq, ydst, ydst[i * 128:(i + 1) * 128, :], hb, hb[:])
        for half in range(2):
            tr = trs[half]
            for kk in range(8):
                k = half * 8 + kk
                fw.issue(fw.pe, lambda k=k, kk=kk, tr=tr: nc.tensor.transpose(tr[:, kk * 128:(kk + 1) * 128],
                                                                             hb[:, k * 128:(k + 1) * 128], idb[:]),
                         reads=[hb, idb], writes=[tr])
            dst = hT[:, half * 8:(half + 1) * 8, i * 128:(i + 1) * 128]
            src = tr[:].rearrange("p (k t) -> p k t", k=8)
            if half == 0:
                fw.issue(fw.act, lambda dst=dst, src=src: nc.scalar.copy(out=dst, in_=src), reads=[tr], writes=[hT])
            else:
                fw.issue(fw.dve, lambda dst=dst, src=src: nc.vector.tensor_copy(out=dst, in_=src), reads=[tr], writes=[hT])


def emit_r1(fw, xsrc, cT, ada_w, ada_b, norm_mix, w_in, modh, modd, pj_tok, pj_feat, pj_gate):
    nc = fw.nc
    emit_mod(fw, cT, ada_w, ada_b, modh)
    allgather(fw, alias(modh, [48, 128]), alias(modd, [96, 128]), 48)
    with ExitStack() as es:
        fw.es = es
        idb = make_identity(fw, "r1_idb", BF16)
        hT = fw.sb("r1_hT", [128, 16, NT], BF16)
        with ExitStack() as es2:
            fw.es = es2
            emit_norm_mod(fw, xsrc, modd, 0, D, norm_mix, hT, idb, nt_tiles=NT // 128)
            fw.barrier()
        fw.es = es
        wts = [fw.sb(f"r1_w{i}", [128, 16, 512], BF16) for i in range(2)]
        stb = [fw.sb(f"r1_st{i}", [128, 512], BF16) for i in range(4)]
        stf = [fw.sb(f"r1_sf{i}", [128, 512], F32) for i in range(4)]
        pss = [fw.ps(f"r1_p{i}", [128, 512], F32) for i in range(4)]
        wv = w_in.h.rearrange("(k p) j -> p k j", p=128)
        plan = [("tok", n * 512, 512, n * 512) for n in range(10)] + [("tok", 5120, 16, 5120)]
        plan += [("feat", 5136, 512, 0), ("feat", 5648, 512, 512), ("gate", 6160, 16, 0)]
        cnt = 0
        for n, (kind, c0, ncol, d0) in enumerate(plan):
            w = wts[n % 2]
            sts = stf if kind == "gate" else stb
            fw.dma(fw.gq, w, w[:, :, 0:ncol], w_in, wv[:, :, c0:c0 + ncol])
            if kind == "feat":
                for fc in range(4):
                    for th in range(NT // 512):
                        p = pss[cnt % 4]; st = sts[cnt % 4]
                        for k in range(16):
                            fw.issue(fw.pe, lambda: nc.tensor.matmul(
                                p[:, :], lhsT=w[:, k, fc * 128:(fc + 1) * 128], rhs=hT[:, k, th * 512:(th + 1) * 512],
                                start=(k == 0), stop=(k == 15)), reads=[w, hT], writes=[p])
                        if cnt % 2 == 0:
                            fw.issue(fw.act, lambda: nc.scalar.copy(out=st[:], in_=p[:]), reads=[p], writes=[st])
                        else:
                            fw.issue(fw.dve, lambda: nc.vector.tensor_copy(out=st[:], in_=p[:]), reads=[p], writes=[st])
                        r0 = d0 + fc * 128
                        fw.dma(fw.sp, pj_feat, pj_feat[r0:r0 + 128, th * 512:(th + 1) * 512], st, st[:])
                        cnt += 1
            else:
                dst = pj_tok if kind == "tok" else pj_gate
                for i in range(NT // 128):
                    p = pss[cnt % 4]; st = sts[cnt % 4]
                    for k in range(16):
                        fw.issue(fw.pe, lambda: nc.tensor.matmul(
                            p[:, 0:ncol], lhsT=hT[:, k, i * 128:(i + 1) * 128], rhs=w[:, k, 0:ncol],
                            start=(k == 0), stop=(k == 15)), reads=[w, hT], writes=[p])
                    if cnt % 2 == 0:
                        fw.issue(fw.act, lambda: nc.scalar.copy(out=st[:, 0:ncol], in_=p[:, 0:ncol]), reads=[p], writes=[st])
                    else:
                        fw.issue(fw.dve, lambda: nc.vector.tensor_copy(out=st[:, 0:ncol], in_=p[:, 0:ncol]), reads=[p], writes=[st])
                    fw.dma(fw.sp, dst, dst[i * 128:(i + 1) * 128, d0:d0 + ncol], st, st[:, 0:ncol])
                    cnt += 1
        fw.barrier()
        fw.es = None


def allgather(fw, src, dst, rpc):
    nc = fw.nc
    rg = [[0, 1], [2, 3], [4, 5], [6, 7]]
    R = src.h.shape[0]
    assert R % rpc == 0
    fw.barrier()
    for k in range(R // rpc):
        fw.issue(fw.cc, lambda: nc.gpsimd.collective_compute("AllGather", ALU.bypass, replica_groups=rg,
                                                             ins=[src.h[k * rpc:(k + 1) * rpc, :]],
                                                             outs=[dst.h[k * 2 * rpc:(k + 1) * 2 * rpc, :]]),
                 reads=[src], writes=[dst])
    fw.barrier()


def gather_rows(fw, out_t, out_ap, table, idx_t, col):
    nc = fw.nc
    return fw.issue(fw.gq, lambda: nc.gpsimd.indirect_dma_start(out=out_ap, out_offset=None, in_=table.h[:, :],
                                                                in_offset=bass.IndirectOffsetOnAxis(ap=idx_t[:, col:col + 1], axis=0)),
                    reads=[table, idx_t], writes=[out_t])


def emit_mix_attn(fw, tabs, prm, cst, merged):
    nc = fw.nc
    NTL = S // 128
    with ExitStack() as es:
        fw.es = es
        qkT = fw.sb("ma_qkT", [128, 8, S], BF16)
        Vaug = fw.sb("ma_V", [128, NTL, 8, 65], BF16)
        mk = fw.sb("ma_mk", [128, 16, 128], BF16)
        cm = fw.sb("ma_cm", [128, 128], BF16)
        BT = fw.sb("ma_BT", [128, 4, 16, 16], F32)
        c3 = fw.sb("ma_c3", [128, 3, 128], F32)
        ones = fw.sb("ma_ones", [128, 128], F32)
        fw.dma(fw.sp, c3, c3[:], cst["c3"], cst["c3"][:])
        fw.issue(fw.pool, lambda: nc.gpsimd.memset(ones[:], 1.0), writes=[ones])
        fw.issue(fw.pool, lambda: nc.gpsimd.memset(Vaug[:, :, :, 64:65], 1.0), writes=[Vaug])
        fw.issue(fw.dve, lambda: nc.vector.tensor_copy(out=cm[:], in_=c3[:, 0, :]), reads=[c3], writes=[cm])
        with ExitStack() as es1:
            fw.es = es1
            idb = make_identity(fw, "ma_idb", BF16)
            mstage = fw.sb("ma_mst", [128, 16, 128], F32)
            fw.dma(fw.sp, mstage, mstage[:], cst["dmask"], cst["dmask"][:])
            fw.issue(fw.dve, lambda: nc.vector.tensor_copy(out=mk[:], in_=mstage[:]), reads=[mstage], writes=[mk])
            G4 = fw.sb("ma_G4", [128, 4, 64], F32)
            for w, nm in enumerate(("fox_qn", "fox_kn", "dil_qn", "dil_kn")):
                gv = prm[nm].h.rearrange("(o j) -> o j", o=1)
                fw.dma(fw.sp, G4, G4[:, w, :], prm[nm], gv[:, :].partition_broadcast(128))
            for w in (0, 2):
                fw.issue(fw.dve, lambda: nc.vector.tensor_scalar_mul(out=G4[:, w, :], in0=G4[:, w, :], scalar1=0.125),
                         reads=[G4], writes=[G4])
            fb = fw.sb("ma_fb", [128, 4], F32)
            fbv = prm["fox_fb"].h.rearrange("(o j) -> o j", o=1)
            fw.dma(fw.sp, fb, fb[:], prm["fox_fb"], fbv[:, :].partition_broadcast(128))
            ffT = fw.sb("ma_ffT", [128, NTL, 4], F32)
            gts = fw.sb("ma_gts", [128, NTL, 8], F32)
            for i in range(NTL):
                gather_rows(fw, gts, gts[:, i, :], tabs["gate"], tabs["idx"], 16 + i)
            fw.issue(fw.dve, lambda: nc.vector.tensor_tensor(out=ffT[:], in0=gts[:, :, 0:4], in1=fb[:].unsqueeze(1).to_broadcast([128, NTL, 4]),
                                                             op=ALU.add), reads=[gts, fb], writes=[ffT])
            fw.issue(fw.act, lambda: nc.scalar.activation(out=ffT[:], in_=ffT[:], func=AF.Exp, scale=-1.0), reads=[ffT], writes=[ffT])
            fw.issue(fw.act, lambda: nc.scalar.activation(out=ffT[:], in_=ffT[:], func=AF.Ln, bias=1.0), reads=[ffT], writes=[ffT])
            pF1 = fw.ps("ma_pF1", [128, 512], F32)
            pF2 = fw.ps("ma_pF2", [128, 512], F32)
            lf2 = ffT[:].rearrange("p i c -> p (i c)")
            fw.issue(fw.pe, lambda: nc.tensor.matmul(pF1[:, 0:64], lhsT=c3[:, 0, :], rhs=lf2, start=True, stop=True),
                     reads=[c3, ffT], writes=[pF1])
            fw.issue(fw.pe, lambda: nc.tensor.matmul(pF2[:, 0:64], lhsT=ones[:], rhs=lf2, start=True, stop=True),
                     reads=[ones, ffT], writes=[pF2])
            tot = fw.sb("ma_tot", [128, NTL, 4], F32)
            offs = fw.sb("ma_offs", [128, NTL, 4], F32)
            Fp = fw.sb("ma_Fp", [128, NTL, 4], F32)
            FR = fw.sb("ma_FR", [128, NTL, 4], F32)
            fw.issue(fw.dve, lambda: nc.vector.tensor_copy(out=tot[:].rearrange("p i c -> p (i c)"), in_=pF2[:, 0:64]),
                     reads=[pF2], writes=[tot])
            fw.issue(fw.dve, lambda: nc.vector.memset(offs[:, 0, :], 0.0), writes=[offs])
            for j in range(1, NTL):
                fw.issue(fw.dve, lambda: nc.vector.tensor_tensor(out=offs[:, j, :], in0=offs[:, j - 1, :], in1=tot[:, j - 1, :], op=ALU.add),
                         reads=[offs, tot], writes=[offs])
            fw.issue(fw.dve, lambda: nc.vector.tensor_tensor(out=Fp[:].rearrange("p i c -> p (i c)"), in0=pF1[:, 0:64],
                                                             in1=offs[:].rearrange("p i c -> p (i c)"), op=ALU.add),
                     reads=[pF1, offs], writes=[Fp])
            fw.issue(fw.pe, lambda: nc.tensor.matmul(pF2[:, 0:64], lhsT=c3[:, 1, :], rhs=Fp[:].rearrange("p i c -> p (i c)"),
                                                     start=True, stop=True), reads=[c3, Fp], writes=[pF2])
            fw.issue(fw.dve, lambda: nc.vector.tensor_copy(out=FR[:].rearrange("p i c -> p (i c)"), in_=pF2[:, 0:64]),
                     reads=[pF2], writes=[FR])
            for i in range(NTL):
                fw.issue(fw.dve, lambda: nc.vector.tensor_tensor(
                    out=BT[:, :, i, :], in0=Fp[:].rearrange("p j h -> p h j"),
                    in1=FR[:, i, :].unsqueeze(2).to_broadcast([128, 4, NTL]), op=ALU.subtract),
                    reads=[Fp, FR], writes=[BT])
            RTs = [fw.sb(f"ma_RT{i}", [128, 2568], BF16) for i in range(4)]
            css = [fw.sb(f"ma_cs{i}", [128, 64], F32) for i in range(2)]
            tmp = fw.sb("ma_tmp", [128, 1024], F32)
            QKn = fw.sb("ma_QKn", [128, 1024], F32)
            QKb = fw.sb("ma_QKb", [128, 1024], BF16)
            rA = fw.sb("ma_rA", [128, 8, 32], F32)
            rB = fw.sb("ma_rB", [128, 8, 32], F32)
            rC = fw.sb("ma_rC", [128, 8, 32], F32)
            rD = fw.sb("ma_rD", [128, 8, 32], F32)
            st = fw.sb("ma_st", [128, 32], F32)
            trp = [fw.ps(f"ma_tr{i}", [128, 1024], BF16) for i in range(2)]
            for i in range(NTL):
                RT = RTs[i % 4]; cs = css[i % 2]; tr = trp[i % 2]
                rows = slice(i * 128, (i + 1) * 128)
                if i == 0:
                    for i2 in range(3):
                        gather_rows(fw, RTs[i2 % 4], RTs[i2 % 4][:, :], tabs["tok"], tabs["idx"], i2)
                if i + 3 < NTL:
                    gather_rows(fw, RTs[(i + 3) % 4], RTs[(i + 3) % 4][:, :], tabs["tok"], tabs["idx"], i + 3)
                fw.dma(fw.sp, cs, cs[:], cst["cs"], cst["cs"][rows, :])
                fw.issue(fw.act, lambda: nc.scalar.activation(out=tmp[:], in_=RT[:, 0:1024], func=AF.Square), reads=[RT], writes=[tmp])
                fw.issue(fw.dve, lambda: nc.vector.tensor_reduce(out=st[:, 0:16], in_=tmp[:].rearrange("p (g d) -> p g d", d=64),
                                                                 axis=AX.X, op=ALU.add), reads=[tmp], writes=[st])
                rstd_from_ss(fw, st, 1.0 / 64, c0=0, c1=16, w=16)
                fw.issue(fw.dve, lambda: nc.vector.tensor_tensor(
                    out=QKn[:].rearrange("p (g d) -> p g d", d=64), in0=RT[:, 0:1024].rearrange("p (g d) -> p g d", d=64),
                    in1=st[:, 16:32].unsqueeze(2).to_broadcast([128, 16, 64]), op=ALU.mult), reads=[RT, st], writes=[QKn])
                fw.issue(fw.pool, lambda: nc.gpsimd.tensor_tensor(
                    out=QKb[:, 0:512].rearrange("p (w h d) -> p w h d", w=2, d=64),
                    in0=QKn[:, 0:512].rearrange("p (w h d) -> p w h d", w=2, d=64),
                    in1=G4[:, 0:2, :].unsqueeze(2).to_broadcast([128, 2, 4, 64]), op=ALU.mult), reads=[QKn, G4], writes=[QKb])
                fw.issue(fw.dve, lambda: nc.vector.tensor_tensor(
                    out=QKn[:, 512:1024].rearrange("p (w h d) -> p w h d", w=2, d=64),
                    in0=QKn[:, 512:1024].rearrange("p (w h d) -> p w h d", w=2, d=64),
                    in1=G4[:, 2:4, :].unsqueeze(2).to_broadcast([128, 2, 4, 64]), op=ALU.mult), reads=[QKn, G4], writes=[QKn])
                dn = QKn[:, 512:1024].rearrange("p (g two d) -> p g two d", two=2, d=32)
                db = QKb[:, 512:1024].rearrange("p (g two d) -> p g two d", two=2, d=32)
                cosb = cs[:, 0:32].unsqueeze(1).to_broadcast([128, 8, 32])
                sinb = cs[:, 32:64].unsqueeze(1).to_broadcast([128, 8, 32])
                fw.issue(fw.dve, lambda: nc.vector.tensor_tensor(out=rA[:], in0=dn[:, :, 0, :], in1=cosb, op=ALU.mult), reads=[QKn, cs], writes=[rA])
                fw.issue(fw.pool, lambda: nc.gpsimd.tensor_tensor(out=rB[:], in0=dn[:, :, 1, :], in1=sinb, op=ALU.mult), reads=[QKn, cs], writes=[rB])
                fw.issue(fw.dve, lambda: nc.vector.tensor_tensor(out=rC[:], in0=dn[:, :, 1, :], in1=cosb, op=ALU.mult), reads=[QKn, cs], writes=[rC])
                fw.issue(fw.pool, lambda: nc.gpsimd.tensor_tensor(out=rD[:], in0=dn[:, :, 0, :], in1=sinb, op=ALU.mult), reads=[QKn, cs], writes=[rD])
                fw.issue(fw.dve, lambda: nc.vector.tensor_tensor(out=db[:, :, 0, :], in0=rA[:], in1=rB[:], op=ALU.subtract), reads=[rA, rB], writes=[QKb])
                fw.issue(fw.dve, lambda: nc.vector.tensor_tensor(out=db[:, :, 1, :], in0=rC[:], in1=rD[:], op=ALU.add), reads=[rC, rD], writes=[QKb])
                for m in range(8):
                    fw.issue(fw.pe, lambda: nc.tensor.transpose(tr[:, m * 128:(m + 1) * 128], QKb[:, m * 128:(m + 1) * 128], idb[:]),
                             reads=[QKb, idb], writes=[tr])
                fw.issue(fw.act, lambda: nc.scalar.copy(out=qkT[:, :, rows], in_=tr[:].rearrange("p (m t) -> p m t", m=8)),
                         reads=[tr], writes=[qkT])
                fw.issue(fw.pool, lambda: nc.gpsimd.tensor_copy(out=Vaug[:, i, :, 0:64], in_=RT[:, 1024:1536].rearrange("p (h d) -> p h d", d=64)),
                         reads=[RT], writes=[Vaug])
            fw.barrier()
        fw.es = es
        pS = [fw.ps(f"ma_pS{i}", [128, 512], F32) for i in range(3)]
        pO = [fw.ps(f"ma_pO{i}", [128, 128], F32) for i in range(4)]
        NPT = 8
        PTs = [fw.sb(f"ma_PT{i}", [128, 128], BF16) for i in range(NPT)]
        PMs = [fw.sb(f"ma_PM{i}", [128, 128], BF16) for i in range(NPT)]
        osb = [fw.sb(f"ma_o{i}", [128, 4, 64], F32) for i in range(2)]
        onb = [fw.sb(f"ma_on{i}", [128, 4, 64], BF16) for i in range(2)]
        sq = fw.sb("ma_sq", [128, 256], F32)
        est = [fw.sb(f"ma_est{i}", [128, 16], F32) for i in range(2)]
        steps = []
        for I in range(NTL // 4):
            for hh in range(8):
                for j in range(4 * I + 4):
                    steps.append((I, hh, j))
        nsteps = len(steps)
        cnt = dict(m=0)

        def stage_s(n):
            I, hh, j = steps[n]
            typ, h = hh // 4, hh % 4
            hp, hl = h // 2, h % 2
            qsel = (0 if typ == 0 else 2) * 2 + hp
            ksel = (1 if typ == 0 else 3) * 2 + hp
            pr = slice(hl * 64, hl * 64 + 64)
            ps = pS[n % 3]
            fw.issue(fw.pe, lambda: nc.tensor.matmul(ps[:], lhsT=qkT[pr, ksel, j * 128:(j + 1) * 128],
                                                     rhs=qkT[pr, qsel, I * 512:(I + 1) * 512], start=True, stop=True),
                     reads=[qkT], writes=[ps])

        def epilogue(u, I, hh):
            o = osb[u % 2]; on = onb[u % 2]; e = est[u % 2]
            for il in range(4):
                po = pO[il]
                fw.issue(fw.dve, lambda: nc.vector.reciprocal(out=e[:, il:il + 1], in_=po[:, 64:65]), reads=[po], writes=[e])
                fw.issue(fw.dve, lambda: nc.vector.tensor_scalar_mul(out=o[:, il, :], in0=po[:, 0:64], scalar1=e[:, il:il + 1]), reads=[po, e], writes=[o])
            fw.issue(fw.act, lambda: nc.scalar.activation(out=sq[:], in_=o[:].rearrange("p h d -> p (h d)"), func=AF.Square), reads=[o], writes=[sq])
            fw.issue(fw.dve, lambda: nc.vector.tensor_reduce(out=e[:, 4:8], in_=sq[:].rearrange("p (h d) -> p h d", d=64), axis=AX.X, op=ALU.add),
                     reads=[sq], writes=[e])
            rstd_from_ss(fw, e, 1.0 / 64, c0=4, c1=8, w=4)
            fw.issue(fw.dve, lambda: nc.vector.tensor_tensor(out=on[:], in0=o[:], in1=e[:, 8:12].unsqueeze(2).to_broadcast([128, 4, 64]), op=ALU.mult),
                     reads=[o, e], writes=[on])
            fw.dma(fw.sp, merged, merged.h[I * 512:(I + 1) * 512, hh * 64:(hh + 1) * 64].rearrange("(i p) d -> p i d", p=128), on, on[:])

        def stage_e(n):
            I, hh, j = steps[n]
            typ, h = hh // 4, hh % 4
            u = I * 8 + hh
            ps = pS[n % 3]
            todo = []
            for il in range(4):
                i = 4 * I + il
                if i < j:
                    continue
                m = cnt["m"]; cnt["m"] += 1
                PT = PTs[m % NPT]; PM = PMs[m % NPT]
                src = ps[:, il * 128:(il + 1) * 128]
                if typ == 0:
                    fw.issue(fw.act, lambda: nc.scalar.activation(out=PT[:], in_=src, func=AF.Exp, bias=BT[:, h, i, j:j + 1]),
                             reads=[ps, BT], writes=[PT])
                    if j == i:
                        fw.issue(fw.dve, lambda: nc.vector.tensor_tensor(out=PM[:], in0=PT[:], in1=cm[:], op=ALU.mult), reads=[PT, cm], writes=[PM])
                        P = PM
                    else:
                        P = PT
                else:
                    fw.issue(fw.act, lambda: nc.scalar.activation(out=PT[:], in_=src, func=AF.Exp), reads=[ps], writes=[PT])
                    eng = fw.dve if (m % 3 != 2) else fw.pool
                    fw.issue(eng, lambda: eng.obj.tensor_tensor(out=PM[:], in0=PT[:], in1=mk[:, i - j, :], op=ALU.mult), reads=[PT, mk], writes=[PM])
                    P = PM
                todo.append((il, i, P))
            for il, i, P in todo:
                po = pO[il]
                fw.issue(fw.pe, lambda: nc.tensor.matmul(po[:, 0:65], lhsT=P[:], rhs=Vaug[:, j, hh, :], start=(j == 0), stop=(j == i)),
                         reads=[P, Vaug], writes=[po])
            if j == 4 * I + 3:
                epilogue(u, I, hh)

        LA = 2
        for n in range(min(LA, nsteps)):
            stage_s(n)
        for n in range(nsteps):
            if n + LA < nsteps:
                stage_s(n + LA)
            stage_e(n)
        fw.barrier()
        fw.es = None


def emit_mix_mlstm(fw, tabs, prm, cst, merged):
    nc = fw.nc
    NCH = S // 128
    with ExitStack() as es:
        fw.es = es
        c3 = fw.sb("ml_c3", [128, 3, 128], F32)
        ones = fw.sb("ml_ones", [128, 128], F32)
        cm = fw.sb("ml_cm", [128, 128], F32)
        idb = make_identity(fw, "ml_idb", BF16)
        fw.dma(fw.sp, c3, c3[:], cst["c3"], cst["c3"][:])
        fw.issue(fw.pool, lambda: nc.gpsimd.memset(ones[:], 1.0), writes=[ones])
        qkT = fw.sb("ml_qkT", [128, 4, S], BF16)
        Vm = fw.sb("ml_V", [128, NCH, 2, 257], BF16)
        fw.issue(fw.pool, lambda: nc.gpsimd.memset(Vm[:, :, :, 256:257], 1.0), writes=[Vm])
        SG = fw.sb("ml_SG", [128, NCH, 512], BF16)
        ig = fw.sb("ml_ig", [128, NCH, 2], F32)
        lf = fw.sb("ml_lf", [128, NCH, 2], F32)
        bm = fw.sb("ml_bm", [128, NCH, 2], F32)
        gb = fw.sb("ml_gb", [128, 4], F32)
        ibv = prm["mlstm_ib"].h.rearrange("(o j) -> o j", o=1)
        fbv = prm["mlstm_fb"].h.rearrange("(o j) -> o j", o=1)
        fw.dma(fw.sp, gb, gb[:, 0:2], prm["mlstm_ib"], ibv[:, :].partition_broadcast(128))
        fw.dma(fw.sp, gb, gb[:, 2:4], prm["mlstm_fb"], fbv[:, :].partition_broadcast(128))
        gts = fw.sb("ml_gts", [128, NCH, 8], F32)
        for i in range(NCH):
            gather_rows(fw, gts, gts[:, i, :], tabs["gate"], tabs["idx"], 16 + i)
        fw.issue(fw.dve, lambda: nc.vector.tensor_tensor(out=ig[:], in0=gts[:, :, 4:6], in1=gb[:, 0:2].unsqueeze(1).to_broadcast([128, NCH, 2]), op=ALU.add),
                 reads=[gts, gb], writes=[ig])
        fw.issue(fw.dve, lambda: nc.vector.tensor_tensor(out=lf[:], in0=gts[:, :, 6:8], in1=gb[:, 2:4].unsqueeze(1).to_broadcast([128, NCH, 2]), op=ALU.add),
                 reads=[gts, gb], writes=[lf])
        fw.issue(fw.act, lambda: nc.scalar.activation(out=lf[:], in_=lf[:], func=AF.Exp, scale=-1.0), reads=[lf], writes=[lf])
        fw.issue(fw.act, lambda: nc.scalar.activation(out=lf[:], in_=lf[:], func=AF.Ln, bias=1.0), reads=[lf], writes=[lf])
        fw.issue(fw.dve, lambda: nc.vector.tensor_scalar_mul(out=lf[:], in0=lf[:], scalar1=-1.0), reads=[lf], writes=[lf])
        with ExitStack() as es1:
            fw.es = es1
            pb = fw.ps("ml_pb", [128, 512], F32)
            fw.issue(fw.pe, lambda: nc.tensor.matmul(pb[:, 0:2 * NCH], lhsT=c3[:, 0, :], rhs=lf[:].rearrange("p i c -> p (i c)"),
                                                     start=True, stop=True), reads=[c3, lf], writes=[pb])
            fw.issue(fw.dve, lambda: nc.vector.tensor_tensor(out=bm[:].rearrange("p i c -> p (i c)"), in0=ig[:].rearrange("p i c -> p (i c)"),
                                                             in1=pb[:, 0:2 * NCH], op=ALU.subtract), reads=[ig, pb], writes=[bm])
            cw = fw.sb("ml_cw", [128, 4, 4], F32)
            fw.dma(fw.sp, cw, cw[:], prm["convT"], prm["convT"].h.rearrange("(m p) j -> p m j", p=128))
            raws = [fw.sb(f"ml_raw{i}", [128, S], BF16) for i in range(2)]
            acc = fw.sb("ml_acc", [128, S], F32)
            for m in range(4):
                raw = raws[m % 2]
                for r_ in range(2):
                    gather_rows(fw, raw, raw[:, r_ * 1024:(r_ + 1) * 1024], tabs["feat"], tabs["idxf"], r_ * 4 + m)
                fw.issue(fw.dve, lambda: nc.vector.tensor_scalar_mul(out=acc[:], in0=raw[:], scalar1=cw[:, m, 3:4]), reads=[raw, cw], writes=[acc])
                for sh in (1, 2, 3):
                    fw.issue(fw.dve, lambda: nc.vector.scalar_tensor_tensor(out=acc[:, sh:], in0=raw[:, 0:S - sh], scalar=cw[:, m, 3 - sh:4 - sh],
                                                                            in1=acc[:, sh:], op0=ALU.mult, op1=ALU.add),
                             reads=[raw, cw, acc], writes=[acc])
                if m < 2:
                    fw.issue(fw.act, lambda: nc.scalar.activation(out=qkT[:, m, :], in_=acc[:], func=AF.Silu), reads=[acc], writes=[qkT])
                else:
                    fw.issue(fw.act, lambda: nc.scalar.activation(out=acc[:], in_=acc[:], func=AF.Silu), reads=[acc], writes=[acc])
                    fw.issue(fw.dve, lambda: nc.vector.tensor_scalar_mul(out=qkT[:, m, :], in0=acc[:], scalar1=128 ** -0.5), reads=[acc], writes=[qkT])
            vfs = [fw.sb(f"ml_vf{i}", [128, 2568], BF16) for i in range(4)]
            for c in range(NCH):
                vf = vfs[c % 4]
                if c == 0:
                    for c2_ in range(3):
                        gather_rows(fw, vfs[c2_ % 4], vfs[c2_ % 4][:, :], tabs["tok"], tabs["idx"], c2_)
                if c + 3 < NCH:
                    gather_rows(fw, vfs[(c + 3) % 4], vfs[(c + 3) % 4][:, :], tabs["tok"], tabs["idx"], c + 3)
                eng = fw.pool if c % 2 == 0 else fw.dve
                fw.issue(eng, lambda: eng.obj.tensor_copy(out=Vm[:, c, :, 0:256], in_=vf[:, 1536:2048].rearrange("p (h d) -> p h d", d=256)), reads=[vf], writes=[Vm])
                fw.issue(fw.act, lambda: nc.scalar.activation(out=SG[:, c, :], in_=vf[:, 2048:2560], func=AF.Sigmoid), reads=[vf], writes=[SG])
            fw.barrier()
        fw.es = es
        fw.issue(fw.dve, lambda: nc.vector.tensor_copy(out=cm[:], in_=c3[:, 0, :]), reads=[c3], writes=[cm])
        pB = fw.ps("ml_pB", [128, 4, 128], F32)
        pSs = fw.ps("ml_pS", [128, 4, 128], F32)
        pND = [fw.ps(f"ml_pND{i}", [128, 512], F32) for i in range(2)]
        pDC = [fw.ps(f"ml_pDC{i}", [128, 512], F32) for i in range(2)]
        pTR = fw.ps("ml_pTR", [128, 8, 128], BF16)
        CT = [fw.sb(f"ml_CT{h}", [128, 257], F32) for h in range(2)]
        CTb = [fw.sb(f"ml_CTb{h}", [128, 257], BF16) for h in range(2)]
        NR = 6
        TL = [fw.sb(f"ml_TL{i}", [128, 128], F32) for i in range(NR)]
        ET = [fw.sb(f"ml_ET{i}", [128, 128], F32) for i in range(NR)]
        EB = [fw.sb(f"ml_EB{i}", [128, 128], F32) for i in range(NR)]
        EM = [fw.sb(f"ml_EM{i}", [128, 128], F32) for i in range(NR)]
        WT = [fw.sb(f"ml_WT{i}", [128, 128], BF16) for i in range(NR)]
        QS = [fw.sb(f"ml_QS{i}", [128, 128], BF16) for i in range(NR)]
        KW = [fw.sb(f"ml_KW{i}", [128, 128], BF16) for i in range(NR)]
        hm = [fw.sb(f"ml_hm{i}", [128, 256], F32) for i in range(NR)]
        ho = [fw.sb(f"ml_ho{i}", [128, 256], BF16) for i in range(NR)]
        sq = fw.sb("ml_sq", [128, 256], F32)
        st = [fw.sb(f"ml_st{i}", [128, 4], F32) for i in range(NR)]
        def part_p(n):
            c, h = n // 2, n % 2
            cs_ = slice(c * 128, (c + 1) * 128)
            r = n % NR; sl = n % 4
            tl = TL[r]; et = ET[r]; eb = EB[r]; em = EM[r]; wt = WT[r]; qs = QS[r]; kw = KW[r]
            fw.issue(fw.dve, lambda: nc.vector.tensor_scalar_mul(out=tl[:], in0=c3[:, 0, :], scalar1=lf[:, c, h:h + 1]), reads=[c3, lf], writes=[tl])
            fw.issue(fw.pe, lambda: nc.tensor.matmul(pB[:, sl, :], lhsT=ones[:], rhs=tl[:], start=True, stop=True), reads=[ones, tl], writes=[pB])
            fw.issue(fw.act, lambda: nc.scalar.activation(out=et[:], in_=pB[:, sl, :], func=AF.Exp, bias=bm[:, c, h:h + 1]), reads=[pB, bm], writes=[et])
            fw.issue(fw.act, lambda: nc.scalar.activation(out=eb[:], in_=pB[:, sl, :], func=AF.Exp), reads=[pB], writes=[eb])
            fw.issue(fw.pe, lambda: nc.tensor.matmul(pSs[:, sl, :], lhsT=qkT[:, 2 + h, cs_], rhs=qkT[:, h, cs_], start=True, stop=True),
                     reads=[qkT], writes=[pSs])
            fw.issue(fw.pool, lambda: nc.gpsimd.tensor_tensor(out=em[:], in0=et[:], in1=cm[:], op=ALU.mult), reads=[et, cm], writes=[em])
            fw.issue(fw.dve, lambda: nc.vector.tensor_tensor(out=wt[:], in0=pSs[:, sl, :], in1=em[:], op=ALU.mult), reads=[pSs, em], writes=[wt])
            fw.issue(fw.pool, lambda: nc.gpsimd.tensor_tensor(out=qs[:], in0=qkT[:, h, cs_], in1=eb[:], op=ALU.mult), reads=[qkT, eb], writes=[qs])
            if c < NCH - 1:
                fw.issue(fw.pe, lambda: nc.tensor.transpose(pTR[:, sl, :], qkT[:, 2 + h, cs_], idb[:]), reads=[qkT, idb], writes=[pTR])
                fw.issue(fw.dve, lambda: nc.vector.tensor_scalar_mul(out=kw[:], in0=pTR[:, sl, :], scalar1=et[:, 127:128]), reads=[pTR, et], writes=[kw])

        def part_q(n):
            c, h = n // 2, n % 2
            cs_ = slice(c * 128, (c + 1) * 128)
            r = n % NR
            eb = EB[r]; wt = WT[r]; qs = QS[r]; kw = KW[r]
            pnd = pND[n % 2]
            fw.issue(fw.pe, lambda: nc.tensor.matmul(pnd[:, 0:257], lhsT=wt[:], rhs=Vm[:, c, h, :], start=True, stop=(c == 0)),
                     reads=[wt, Vm], writes=[pnd])
            if c > 0:
                fw.issue(fw.pe, lambda: nc.tensor.matmul(pnd[:, 0:257], lhsT=qs[:], rhs=CTb[h][:], start=False, stop=True),
                         reads=[qs, CTb[h]], writes=[pnd])
            if c < NCH - 1:
                pdc = pDC[n % 2]
                fw.issue(fw.pe, lambda: nc.tensor.matmul(pdc[:, 0:257], lhsT=kw[:], rhs=Vm[:, c, h, :], start=True, stop=True),
                         reads=[kw, Vm], writes=[pdc])
                if c == 0:
                    fw.issue(fw.dve, lambda: nc.vector.tensor_copy(out=CT[h][:], in_=pdc[:, 0:257]), reads=[pdc], writes=[CT[h]])
                else:
                    fw.issue(fw.dve, lambda: nc.vector.scalar_tensor_tensor(out=CT[h][:], in0=CT[h][:], scalar=eb[:, 127:128], in1=pdc[:, 0:257],
                                                                            op0=ALU.mult, op1=ALU.add), reads=[CT[h], eb, pdc], writes=[CT[h]])
                fw.issue(fw.act, lambda: nc.scalar.copy(out=CTb[h][:], in_=CT[h][:]), reads=[CT[h]], writes=[CTb[h]])
            s_ = st[r]
            fw.issue(fw.dve, lambda: nc.vector.tensor_scalar(out=s_[:, 3:4], in0=pnd[:, 256:257], scalar1=-1.0, scalar2=1.0, op0=ALU.mult, op1=ALU.max),
                     reads=[pnd], writes=[s_])
            fw.issue(fw.dve, lambda: nc.vector.tensor_tensor(out=s_[:, 0:1], in0=pnd[:, 256:257], in1=s_[:, 3:4], op=ALU.max), reads=[pnd, s_], writes=[s_])
            fw.issue(fw.dve, lambda: nc.vector.reciprocal(out=s_[:, 0:1], in_=s_[:, 0:1]), reads=[s_], writes=[s_])
            fw.issue(fw.dve, lambda: nc.vector.tensor_scalar_mul(out=hm[r][:], in0=pnd[:, 0:256], scalar1=s_[:, 0:1]), reads=[pnd, s_], writes=[hm[r]])
            fw.issue(fw.dve, lambda: nc.vector.scalar_tensor_tensor(out=sq[:], in0=hm[r][:], scalar=1.0, in1=hm[r][:], op0=ALU.mult, op1=ALU.mult,
                                                                    accum_out=s_[:, 1:2]), reads=[hm[r]], writes=[sq, s_])
            rstd_from_ss(fw, s_, 1.0 / 256, c0=1, c1=2, w=1)
            fw.issue(fw.dve, lambda: nc.vector.scalar_tensor_tensor(out=ho[r][:], in0=hm[r][:], scalar=s_[:, 2:3], in1=SG[:, c, h * 256:(h + 1) * 256],
                                                                    op0=ALU.mult, op1=ALU.mult),
                     reads=[hm[r], s_, SG], writes=[ho[r]])
            fw.dma(fw.sp, merged, merged[cs_, 512 + h * 256:512 + (h + 1) * 256], ho[r], ho[r][:])

        NS = 2 * NCH
        LA = 3
        for n in range(min(LA, NS)):
            part_p(n)
        for n in range(NS):
            if n + LA < NS:
                part_p(n + LA)
            part_q(n)
        fw.barrier()
        fw.es = None


def emit_wout(fw, xsrc, mrg, idxm, modd, out_norm, w_out, x1d):
    nc = fw.nc
    NTL = NT // 128
    with ExitStack() as es:
        fw.es = es
        idb = make_identity(fw, "wo_idb", BF16)
        mT = fw.sb("wo_mT", [128, 16, NT], BF16)
        on_b = fw.sb("wo_on", [128, D], F32)
        g1_b = fw.sb("wo_g1", [128, D], F32)
        mts = [fw.sb(f"wo_mt{i}", [128, D], BF16) for i in range(8)]
        mbs = [fw.sb(f"wo_mb{i}", [128, D], BF16) for i in range(2)]
        trs = [fw.ps(f"wo_tr{i}", [128, 1024], BF16) for i in range(2)]
        mv = modd.h.rearrange("(o j) -> o j", o=1)
        ov = out_norm.h.rearrange("(o j) -> o j", o=1)
        fw.dma(fw.sp, on_b, on_b[:], out_norm, ov[:, :].partition_broadcast(128))
        fw.dma(fw.sp, g1_b, g1_b[:], modd, mv[:, 2 * D:3 * D].partition_broadcast(128))
        for i in range(NTL):
            for r_ in range(2):
                gather_rows(fw, mts[i % 8], mts[i % 8][:, r_ * 1024:(r_ + 1) * 1024], mrg, idxm, r_ * (NT // 128) + i)
        for i in range(NTL):
            mt = mts[i % 8]; mb = mbs[i % 2]
            fw.issue(fw.dve, lambda: nc.vector.tensor_tensor(out=mb[:], in0=mt[:], in1=on_b[:], op=ALU.mult), reads=[mt, on_b], writes=[mb])
            for half in range(2):
                tr = trs[half]
                for kk in range(8):
                    k = half * 8 + kk
                    fw.issue(fw.pe, lambda: nc.tensor.transpose(tr[:, kk * 128:(kk + 1) * 128], mb[:, k * 128:(k + 1) * 128], idb[:]),
                             reads=[mb, idb], writes=[tr])
                dst = mT[:, half * 8:(half + 1) * 8, i * 128:(i + 1) * 128]
                src = tr[:].rearrange("p (k t) -> p k t", k=8)
                if half == 0:
                    fw.issue(fw.act, lambda: nc.scalar.copy(out=dst, in_=src), reads=[tr], writes=[mT])
                else:
                    fw.issue(fw.pool if False else fw.dve, lambda: nc.vector.tensor_copy(out=dst, in_=src), reads=[tr], writes=[mT])
        wts = [fw.sb(f"wo_w{i}", [128, 16, 512], BF16) for i in range(2)]
        xcs = [fw.sb(f"wo_xc{i}", [128, 512], F32) for i in range(4)]
        t1s = [fw.sb(f"wo_t1{i}", [128, 512], F32) for i in range(4)]
        pss = [fw.ps(f"wo_p{i}", [128, 512], F32) for i in range(4)]
        wv = w_out.h.rearrange("(k p) j -> p k j", p=128)
        cnt = 0
        for n in range(4):
            w = wts[n % 2]
            ns = slice(n * 512, (n + 1) * 512)
            fw.dma(fw.gq, w, w[:], w_out, wv[:, :, ns])
            for i in range(NTL):
                p = pss[cnt % 4]; xc = xcs[cnt % 4]; t1 = t1s[cnt % 4]
                rows = slice(i * 128, (i + 1) * 128)
                fw.dma(fw.sp, xc, xc[:], xsrc, xsrc[rows, ns])
                for k in range(16):
                    fw.issue(fw.pe, lambda: nc.tensor.matmul(p[:], lhsT=mT[:, k, rows], rhs=w[:, k, :], start=(k == 0), stop=(k == 15)),
                             reads=[mT, w], writes=[p])
                fw.issue(fw.dve, lambda: nc.vector.tensor_tensor(out=t1[:], in0=p[:], in1=g1_b[:, ns], op=ALU.mult), reads=[p, g1_b], writes=[t1])
                fw.issue(fw.pool, lambda: nc.gpsimd.tensor_tensor(out=t1[:], in0=t1[:], in1=xc[:], op=ALU.add), reads=[t1, xc], writes=[t1])
                fw.dma(fw.sp, x1d, x1d[rows, ns], t1, t1[:])
                cnt += 1
        fw.barrier()
        fw.es = None


def emit_peer_route(fw, yT, wq, k1, k2, cst, idxT, gT, dense=None):
    nc = fw.nc
    NTL = NT // 128
    with ExitStack() as es:
        fw.es = es
        idf = make_identity(fw, "pr_idf", F32)
        iota = fw.sb("pr_iota", [128, 128], F32)
        fw.dma(fw.sp, iota, iota[:], cst["iota"], cst["iota"][:])
        V16 = fw.sb("pr_V16", [128, NTL, 16, 16], F32)
        I16 = fw.sb("pr_I16", [128, NTL, 16, 16], U32)
        kT = fw.sb("pr_kT", [128, 2, 128], F32)
        kst = fw.sb("pr_kst", [128, 2, 128], F32)
        with ExitStack() as es1:
            fw.es = es1
            pk = fw.ps("pr_pk", [128, 2, 128], F32)
            for sd, kk in enumerate((k1, k2)):
                fw.dma(fw.sp, kst, kst[:, sd, :], kk, kk[:])
                fw.issue(fw.pe, lambda: nc.tensor.transpose(pk[:, sd, :], kst[:, sd, :], idf[:]), reads=[kst, idf], writes=[pk])
            fw.issue(fw.act, lambda: nc.scalar.copy(out=kT[:], in_=pk[:]), reads=[pk], writes=[kT])
            wts = [fw.sb(f"pr_w{i}", [128, 16, 512], BF16) for i in range(2)]
            qps = [fw.sb(f"pr_qp{i}", [128, 512], F32) for i in range(2)]
            scs = [fw.sb(f"pr_sc{i}", [128, 128], F32) for i in range(4)]
            wks = [fw.sb(f"pr_wk{i}", [128, 128], F32) for i in range(4)]
            pqs = [fw.ps(f"pr_pq{i}", [128, 512], F32) for i in range(2)]
            pscs = [fw.ps(f"pr_ps{i}", [128, 4, 128], F32) for i in range(2)]
            wv = wq.h.rearrange("(k p) j -> p k j", p=128)
            cnt = 0
            c2 = 0
            for n in range(4):
                w = wts[n % 2]
                fw.dma(fw.gq, w, w[:], wq, wv[:, :, n * 512:(n + 1) * 512])
                for mm in range(4):
                    m = n * 4 + mm
                    side = m % 2
                    for th in range(NT // 512):
                        pq = pqs[cnt % 2]; qp = qps[cnt % 2]
                        for k in range(16):
                            fw.issue(fw.pe, lambda: nc.tensor.matmul(pq[:], lhsT=w[:, k, mm * 128:(mm + 1) * 128], rhs=yT[:, k, th * 512:(th + 1) * 512],
                                                                     start=(k == 0), stop=(k == 15)), reads=[w, yT], writes=[pq])
                        fw.issue(fw.act, lambda: nc.scalar.copy(out=qp[:], in_=pq[:]), reads=[pq], writes=[qp])
                        cnt += 1
                        for tt in range(4):
                            tl_i = th * 4 + tt
                            psc = pscs[(c2 // 4) % 2]; sl = c2 % 4
                            sc = scs[c2 % 4]; wk = wks[c2 % 4]
                            fw.issue(fw.pe, lambda: nc.tensor.matmul(psc[:, sl, :], lhsT=qp[:, tt * 128:(tt + 1) * 128], rhs=kT[:, side, :],
                                                                     start=True, stop=True), reads=[qp, kT], writes=[psc])
                            fw.issue(fw.act, lambda: nc.scalar.copy(out=sc[:], in_=psc[:, sl, :]), reads=[psc], writes=[sc])
                            v = V16[:, tl_i, m, :]; ix = I16[:, tl_i, m, :]
                            fw.issue(fw.dve, lambda: nc.vector.max(out=v[:, 0:8], in_=sc[:]), reads=[sc], writes=[V16])
                            fw.issue(fw.dve, lambda: nc.vector.max_index(out=ix[:, 0:8], in_max=v[:, 0:8], in_values=sc[:]), reads=[sc, V16], writes=[I16])
                            fw.issue(fw.dve, lambda: nc.vector.match_replace(out=wk[:], in_to_replace=v[:, 0:8], in_values=sc[:], imm_value=-1e30),
                                     reads=[sc, V16], writes=[wk])
                            fw.issue(fw.dve, lambda: nc.vector.max(out=v[:, 8:16], in_=wk[:]), reads=[wk], writes=[V16])
                            fw.issue(fw.dve, lambda: nc.vector.max_index(out=ix[:, 8:16], in_max=v[:, 8:16], in_values=wk[:]), reads=[wk, V16], writes=[I16])
                            c2 += 1
            fw.barrier()
        fw.es = es
        cand = fw.sb("pr_cand", [128, 8, 256], F32)
        cwk = fw.sb("pr_cwk", [128, 256], F32)
        tv = fw.sb("pr_tv", [128, 8, 16], F32)
        tc_ = fw.sb("pr_tc", [128, 8, 16], U32)
        hi = fw.sb("pr_hi", [128, 128], I32)
        lo = fw.sb("pr_lo", [128, 128], I32)
        hif = fw.sb("pr_hif", [128, 128], F32)
        lof = fw.sb("pr_lof", [128, 128], F32)
        i1f = fw.sb("pr_i1f", [128, 8, 16], F32)
        i2f = fw.sb("pr_i2f", [128, 8, 16], F32)
        oh = fw.sb("pr_oh", [128, 128, 16], F32)
        e1 = fw.sb("pr_e1", [128, 128], F32)
        e2 = fw.sb("pr_e2", [128, 128], F32)
        eid = fw.sb("pr_eid", [128, 128], F32)
        gg = fw.sb("pr_gg", [128, 8, 16], F32)
        sm = fw.sb("pr_sm", [128, 24], F32)
        ptr = fw.ps("pr_ptr", [128, 2, 128], F32)
        io16 = iota[:, 0:16].unsqueeze(1).to_broadcast([128, 128, 16])
        for i in range(NTL):
            vv = V16[:, i, :, :].rearrange("p (h s) k -> p h s k", s=2)
            ii = I16[:, i, :, :].rearrange("p (h s) k -> p h s k", s=2)
            fw.issue(fw.dve, lambda: nc.vector.tensor_tensor(out=cand[:].rearrange("p h (a b) -> p h a b", b=16),
                                                             in0=vv[:, :, 0, :].unsqueeze(3).to_broadcast([128, 8, 16, 16]),
                                                             in1=vv[:, :, 1, :].unsqueeze(2).to_broadcast([128, 8, 16, 16]), op=ALU.add),
                     reads=[V16], writes=[cand])
            fw.issue(fw.dve, lambda: nc.vector.tensor_copy(out=i1f[:], in_=ii[:, :, 0, :]), reads=[I16], writes=[i1f])
            fw.issue(fw.dve, lambda: nc.vector.tensor_copy(out=i2f[:], in_=ii[:, :, 1, :]), reads=[I16], writes=[i2f])
            for h in range(8):
                fw.issue(fw.dve, lambda: nc.vector.max(out=tv[:, h, 0:8], in_=cand[:, h, :]), reads=[cand], writes=[tv])
                fw.issue(fw.dve, lambda: nc.vector.max_index(out=tc_[:, h, 0:8], in_max=tv[:, h, 0:8], in_values=cand[:, h, :]), reads=[cand, tv], writes=[tc_])
                fw.issue(fw.dve, lambda: nc.vector.match_replace(out=cwk[:], in_to_replace=tv[:, h, 0:8], in_values=cand[:, h, :], imm_value=-1e30),
                         reads=[cand, tv], writes=[cwk])
                fw.issue(fw.dve, lambda: nc.vector.max(out=tv[:, h, 8:16], in_=cwk[:]), reads=[cwk], writes=[tv])
                fw.issue(fw.dve, lambda: nc.vector.max_index(out=tc_[:, h, 8:16], in_max=tv[:, h, 8:16], in_values=cwk[:]), reads=[cwk, tv], writes=[tc_])
            fw.issue(fw.dve, lambda: nc.vector.tensor_scalar_mul(out=sm[:, 0:8], in0=tv[:, :, 0], scalar1=-1.0), reads=[tv], writes=[sm])
            for h in range(8):
                fw.issue(fw.act, lambda: nc.scalar.activation(out=gg[:, h, :], in_=tv[:, h, :], func=AF.Exp, bias=sm[:, h:h + 1], accum_out=sm[:, 8 + h:9 + h]),
                         reads=[tv, sm], writes=[gg, sm])
            fw.issue(fw.dve, lambda: nc.vector.reciprocal(out=sm[:, 16:24], in_=sm[:, 8:16]), reads=[sm], writes=[sm])
            fw.issue(fw.dve, lambda: nc.vector.tensor_tensor(out=gg[:], in0=gg[:], in1=sm[:, 16:24].unsqueeze(2).to_broadcast([128, 8, 16]), op=ALU.mult),
                     reads=[gg, sm], writes=[gg])
            tci = tc_[:].rearrange("p h k -> p (h k)").bitcast(I32)
            fw.issue(fw.dve, lambda: nc.vector.tensor_single_scalar(out=hi[:], in_=tci, scalar=4, op=ALU.arith_shift_right), reads=[tc_], writes=[hi])
            fw.issue(fw.dve, lambda: nc.vector.tensor_single_scalar(out=lo[:], in_=tci, scalar=15, op=ALU.bitwise_and), reads=[tc_], writes=[lo])
            fw.issue(fw.dve, lambda: nc.vector.tensor_copy(out=hif[:], in_=hi[:]), reads=[hi], writes=[hif])
            fw.issue(fw.dve, lambda: nc.vector.tensor_copy(out=lof[:], in_=lo[:]), reads=[lo], writes=[lof])
            for (xf, tab, eo) in ((hif, i1f, e1), (lof, i2f, e2)):
                fw.issue(fw.dve, lambda: nc.vector.tensor_tensor(out=oh[:], in0=xf[:].unsqueeze(2).to_broadcast([128, 128, 16]), in1=io16, op=ALU.is_equal),
                         reads=[xf, iota], writes=[oh])
                fw.issue(fw.pool, lambda: nc.gpsimd.tensor_tensor(out=oh[:].rearrange("p (h k) i -> p h k i", h=8),
                                                                  in0=oh[:].rearrange("p (h k) i -> p h k i", h=8),
                                                                  in1=tab[:].unsqueeze(2).to_broadcast([128, 8, 16, 16]), op=ALU.mult),
                         reads=[oh, tab], writes=[oh])
                fw.issue(fw.dve, lambda: nc.vector.tensor_reduce(out=eo[:], in_=oh[:], axis=AX.X, op=ALU.add), reads=[oh], writes=[eo])
            fw.issue(fw.dve, lambda: nc.vector.scalar_tensor_tensor(out=eid[:], in0=e1[:], scalar=128.0, in1=e2[:], op0=ALU.mult, op1=ALU.add),
                     reads=[e1, e2], writes=[eid])
            if dense is not None:
                e1T, e2T = dense
                fw.issue(fw.pe, lambda: nc.tensor.transpose(ptr[:, 0, :], e1[:], idf[:]), reads=[e1, idf], writes=[ptr])
                fw.issue(fw.pe, lambda: nc.tensor.transpose(ptr[:, 1, :], e2[:], idf[:]), reads=[e2, idf], writes=[ptr])
                fw.issue(fw.dve, lambda: nc.vector.tensor_copy(out=e1T[:, i * 128:(i + 1) * 128], in_=ptr[:, 0, :]), reads=[ptr], writes=[e1T])
                fw.issue(fw.dve, lambda: nc.vector.tensor_copy(out=e2T[:, i * 128:(i + 1) * 128], in_=ptr[:, 1, :]), reads=[ptr], writes=[e2T])
                fw.issue(fw.pe, lambda: nc.tensor.transpose(ptr[:, 1, :], gg[:].rearrange("p h k -> p (h k)"), idf[:]), reads=[gg, idf], writes=[ptr])
                fw.issue(fw.dve, lambda: nc.vector.tensor_copy(out=gT[:, i * 128:(i + 1) * 128], in_=ptr[:, 1, :]), reads=[ptr], writes=[gT])
                continue
            fw.issue(fw.pe, lambda: nc.tensor.transpose(ptr[:, 0, :], eid[:], idf[:]), reads=[eid, idf], writes=[ptr])
            fw.issue(fw.pe, lambda: nc.tensor.transpose(ptr[:, 1, :], gg[:].rearrange("p h k -> p (h k)"), idf[:]), reads=[gg, idf], writes=[ptr])
            fw.issue(fw.dve, lambda: nc.vector.tensor_copy(out=idxT[:, i * 128:(i + 1) * 128], in_=ptr[:, 0, :]), reads=[ptr], writes=[idxT])
            fw.issue(fw.dve, lambda: nc.vector.tensor_copy(out=gT[:, i * 128:(i + 1) * 128], in_=ptr[:, 1, :]), reads=[ptr], writes=[gT])
        fw.barrier()
        fw.es = None


def emit_peer_apply(fw, yd, idxT, gT, u_tab, v_tab, modd, x1d, xout, cst):
    nc = fw.nc
    NTL = NT // 128
    with ExitStack() as es:
        fw.es = es
        iota = fw.sb("pa_iota", [128, 128], F32)
        fw.dma(fw.sp, iota, iota[:], cst["iota"], cst["iota"][:])
        g2_b = fw.sb("pa_g2", [128, D], F32)
        mv = modd.h.rearrange("(o j) -> o j", o=1)
        fw.dma(fw.sp, g2_b, g2_b[:], modd, mv[:, 5 * D:6 * D].partition_broadcast(128))
        NB = 4
        Ub = [fw.sb(f"pa_U{i}", [128, D], BF16) for i in range(NB)]
        Yb = [fw.sb(f"pa_Y{i}", [128, D], BF16) for i in range(NB)]
        junk = fw.sb("pa_junk", [128, D], BF16)
        Wd = [fw.sb(f"pa_Wd{i}", [128, 128], BF16) for i in range(NB)]
        A = [fw.sb(f"pa_A{i}", [128, 128], F32) for i in range(2)]
        W = [fw.sb(f"pa_W{i}", [128, 128], F32) for i in range(2)]
        pO = [fw.ps(f"pa_pO{i}", [128, 512], F32) for i in range(4)]
        xcs = [fw.sb(f"pa_xc{i}", [128, 512], F32) for i in range(2)]
        t1s = [fw.sb(f"pa_t1{i}", [128, 512], F32) for i in range(2)]
        n = 0
        for i in range(NTL):
            a = A[i % 2]; w = W[i % 2]
            for tl in range(128):
                t = i * 128 + tl
                ub = Ub[n % NB]; yb = Yb[n % NB]
                fw.issue(fw.gq, lambda: nc.gpsimd.indirect_dma_start(out=ub[:, :], out_offset=None, in_=u_tab[:, :],
                                                                     in_offset=bass.IndirectOffsetOnAxis(ap=idxT[:, t:t + 1], axis=0)),
                         reads=[u_tab, idxT], writes=[ub])
                fw.dma(fw.sp, yb, yb[:], yd, yd[t:t + 1, :].partition_broadcast(128))
                fw.issue(fw.dve, lambda: nc.vector.scalar_tensor_tensor(out=junk[:], in0=ub[:], scalar=1.0, in1=yb[:], op0=ALU.mult, op1=ALU.mult,
                                                                        accum_out=a[:, tl:tl + 1]), reads=[ub, yb], writes=[junk, a])
                n += 1
            fw.issue(fw.act, lambda: nc.scalar.activation(out=w[:], in_=a[:], func=AF.Gelu), reads=[a], writes=[w])
            fw.issue(fw.dve, lambda: nc.vector.tensor_tensor(out=w[:], in0=w[:], in1=gT[:, i * 128:(i + 1) * 128], op=ALU.mult), reads=[w, gT], writes=[w])
            for tl in range(128):
                t = i * 128 + tl
                vb = Ub[n % NB]; wd = Wd[n % NB]
                fw.issue(fw.gq, lambda: nc.gpsimd.indirect_dma_start(out=vb[:, :], out_offset=None, in_=v_tab[:, :],
                                                                     in_offset=bass.IndirectOffsetOnAxis(ap=idxT[:, t:t + 1], axis=0)),
                         reads=[v_tab, idxT], writes=[vb])
                fw.issue(fw.dve, lambda: nc.vector.tensor_scalar(out=wd[:], in0=iota[:], scalar1=float(tl), scalar2=w[:, tl:tl + 1],
                                                                 op0=ALU.is_equal, op1=ALU.mult), reads=[iota, w], writes=[wd])
                for c in range(4):
                    fw.issue(fw.pe, lambda: nc.tensor.matmul(pO[c][:], lhsT=wd[:], rhs=vb[:, c * 512:(c + 1) * 512], start=(tl == 0), stop=(tl == 127)),
                             reads=[wd, vb], writes=[pO[c]])
                n += 1
            rows = slice(i * 128, (i + 1) * 128)
            for c in range(4):
                xc = xcs[c % 2]; t1 = t1s[c % 2]
                cs_ = slice(c * 512, (c + 1) * 512)
                fw.dma(fw.sp, xc, xc[:], x1d, x1d[rows, cs_])
                fw.issue(fw.dve, lambda: nc.vector.tensor_tensor(out=t1[:], in0=pO[c][:], in1=g2_b[:, cs_], op=ALU.mult), reads=[pO[c], g2_b], writes=[t1])
                fw.issue(fw.pool, lambda: nc.gpsimd.tensor_tensor(out=t1[:], in0=t1[:], in1=xc[:], op=ALU.add), reads=[t1, xc], writes=[t1])
                fw.dma(fw.sp, xout, xout[rows, cs_], t1, t1[:])
        fw.barrier()
        fw.es = None


def emit_peer_G(fw, e1T, e2T, gT, cst, Gd):
    nc = fw.nc
    TB = 256
    with ExitStack() as es:
        fw.es = es
        iota = fw.sb("pg_iota", [128, 128], F32)
        iob = fw.sb("pg_iob", [128, 128], BF16)
        fw.dma(fw.sp, iota, iota[:], cst["iota"], cst["iota"][:])
        fw.issue(fw.dve, lambda: nc.vector.tensor_copy(out=iob[:], in_=iota[:]), reads=[iota], writes=[iob])
        Gs = fw.sb("pg_Gs", [128, 128, TB], BF16)
        NR = 4
        As = [fw.sb(f"pg_A{i}", [128, 4, 128], BF16) for i in range(NR)]
        Bs = [fw.sb(f"pg_B{i}", [128, 4, 128], BF16) for i in range(NR)]
        pG = [fw.ps(f"pg_p{i}", [128, 4, 128], F32) for i in range(2)]
        n = 0
        for tb in range(NT // TB):
            for q in range(TB // 4):
                pg = pG[q % 2]
                t0 = tb * TB + q * 4
                A = As[n % NR]; Bg = Bs[n % NR]
                n += 1
                pgv = pg[:].rearrange("p t e -> p (t e)").rearrange("p (e t) -> p t e", t=4)
                for j in range(4):
                    t = t0 + j
                    fw.issue(fw.dve, lambda: nc.vector.tensor_scalar(out=A[:, j, :], in0=iob[:], scalar1=e1T[:, t:t + 1], scalar2=None, op0=ALU.is_equal),
                             reads=[iob, e1T], writes=[A])
                    fw.issue(fw.dve, lambda: nc.vector.tensor_scalar(out=Bg[:, j, :], in0=iob[:], scalar1=e2T[:, t:t + 1], scalar2=gT[:, t:t + 1],
                                                                     op0=ALU.is_equal, op1=ALU.mult), reads=[iob, e2T, gT], writes=[Bg])
                for j in range(4):
                    fw.issue(fw.pe, lambda: nc.tensor.matmul(pgv[:, j, :], lhsT=Bg[:, j, :], rhs=A[:, j, :], start=True, stop=True), reads=[A, Bg], writes=[pg])
                src = pg[:].rearrange("p t e -> p (t e)").rearrange("p (e t) -> p e t", t=4)
                fw.issue(fw.act, lambda: nc.scalar.copy(out=Gs[:, :, q * 4:q * 4 + 4], in_=src), reads=[pg], writes=[Gs])
            for e0 in range(0, 128, 32):
                fw.dma(fw.sp, Gd, Gd.h[e0:e0 + 32, :, tb * TB:(tb + 1) * TB].rearrange("a b t -> b a t"), Gs, Gs[:, e0:e0 + 32, :])
        fw.barrier()
        fw.es = None


def emit_peer_dense(fw, yT, Gd, u_tab, v_tab, modd, x1d, xout):
    nc = fw.nc
    NTL = NT // 128
    CG = 2
    with ExitStack() as es:
        fw.es = es
        idb = make_identity(fw, "pd_idb", BF16)
        Oacc = [[fw.sb(f"pd_O{tt}_{dc}", [128, 512], F32) for dc in range(4)] for tt in range(NTL)]
        ub = [fw.sb(f"pd_u{i}", [128, CG, D], BF16) for i in range(2)]
        vb = [fw.sb(f"pd_v{i}", [128, CG, D], BF16) for i in range(2)]
        gt = [fw.sb(f"pd_g{i}", [128, CG, NT], BF16) for i in range(2)]
        uT = [fw.sb(f"pd_uT{i}", [128, CG, 16, 128], BF16) for i in range(2)]
        Ga = [fw.sb(f"pd_Ga{i}", [128, NT], BF16) for i in range(2)]
        WT = [fw.sb(f"pd_WT{i}", [128, CG, NT], BF16) for i in range(2)]
        tmp = [fw.sb(f"pd_tmp{i}", [128, 512], F32) for i in range(2)]
        pA = [fw.ps(f"pd_pA{i}", [128, 512], F32) for i in range(2)]
        ptr = [fw.ps(f"pd_ptr{i}", [128, 1024], BF16) for i in range(2)]
        pO = [fw.ps(f"pd_pO{i}", [128, 512], F32) for i in range(4)]
        uv = u_tab.h.rearrange("(c p) d -> p c d", p=128)
        vv = v_tab.h.rearrange("(c p) d -> p c d", p=128)
        NG = 128 // CG
        cnt = dict(na=0, no=0)

        def stage_a(gi):
            b = gi % 2
            c0 = gi * CG
            if gi == 0:
                fw.dma(fw.gq, ub[b], ub[b][:], u_tab, uv[:, c0:c0 + CG, :])
            fw.dma(fw.gq, vb[b], vb[b][:], v_tab, vv[:, c0:c0 + CG, :])
            fw.dma(fw.sp, gt[b], gt[b][:], Gd, Gd.h[c0:c0 + CG, :, :].rearrange("c e t -> e c t"))
            for cc in range(CG):
                for half in range(2):
                    tr = ptr[half]
                    for kk in range(8):
                        k = half * 8 + kk
                        fw.issue(fw.pe, lambda: nc.tensor.transpose(tr[:, kk * 128:(kk + 1) * 128], ub[b][:, cc, k * 128:(k + 1) * 128], idb[:]),
                                 reads=[ub[b], idb], writes=[tr])
                        yield
                    dst = uT[b][:, cc, half * 8:(half + 1) * 8, :]
                    src = tr[:].rearrange("p (k e) -> p k e", k=8)
                    if half == 0:
                        fw.issue(fw.act, lambda: nc.scalar.copy(out=dst, in_=src), reads=[tr], writes=[uT[b]])
                    else:
                        fw.issue(fw.dve, lambda: nc.vector.tensor_copy(out=dst, in_=src), reads=[tr], writes=[uT[b]])
                if cc > 0:
                    yield from a_mm(b, cc - 1)
            if gi + 1 < NG:
                fw.dma(fw.gq, ub[1 - b], ub[1 - b][:], u_tab, uv[:, c0 + CG:c0 + 2 * CG, :])
            yield from a_mm(b, CG - 1)

        def a_mm(b, cc):
            ga = Ga[cc % 2]
            for th in range(NT // 512):
                pa = pA[cnt["na"] % 2]
                cnt["na"] += 1
                for k in range(16):
                    fw.issue(fw.pe, lambda: nc.tensor.matmul(pa[:], lhsT=uT[b][:, cc, k, :], rhs=yT[:, k, th * 512:(th + 1) * 512],
                                                             start=(k == 0), stop=(k == 15)), reads=[uT[b], yT], writes=[pa])
                    yield
                fw.issue(fw.act, lambda: nc.scalar.activation(out=ga[:, th * 512:(th + 1) * 512], in_=pa[:], func=AF.Gelu), reads=[pa], writes=[ga])
            fw.issue(fw.dve, lambda: nc.vector.tensor_tensor(out=WT[b][:, cc, :], in0=ga[:], in1=gt[b][:, cc, :], op=ALU.mult),
                     reads=[ga, gt[b]], writes=[WT[b]])

        def stage_o(gi):
            b = gi % 2
            for tt in range(NTL):
                for dc in range(4):
                    no = cnt["no"]
                    po = pO[no % 4]
                    for cc in range(CG):
                        fw.issue(fw.pe, lambda: nc.tensor.matmul(po[:], lhsT=WT[b][:, cc, tt * 128:(tt + 1) * 128], rhs=vb[b][:, cc, dc * 512:(dc + 1) * 512],
                                                                 start=(cc == 0), stop=(cc == CG - 1)), reads=[WT[b], vb[b]], writes=[po])
                    ot = Oacc[tt][dc]
                    osl = ot[:]
                    if gi == 0:
                        fw.issue(fw.act, lambda: nc.scalar.copy(out=osl, in_=po[:]), reads=[po], writes=[ot])
                    elif no % 4 != 3:
                        fw.issue(fw.dve, lambda: nc.vector.tensor_tensor(out=osl, in0=po[:], in1=osl, op=ALU.add), reads=[po, ot], writes=[ot])
                    else:
                        tm = tmp[(no // 4) % 2]
                        fw.issue(fw.act, lambda: nc.scalar.copy(out=tm[:], in_=po[:]), reads=[po], writes=[tm])
                        fw.issue(fw.pool, lambda: nc.gpsimd.tensor_tensor(out=osl, in0=tm[:], in1=osl, op=ALU.add), reads=[tm, ot], writes=[ot])
                    cnt["no"] += 1
                    yield

        for _ in stage_a(0):
            pass
        for gi in range(NG):
            ga_ = stage_a(gi + 1) if gi + 1 < NG else iter(())
            for _ in stage_o(gi):
                for _k in range(3):
                    next(ga_, None)
            for _ in ga_:
                pass
        g2_b = fw.sb("pd_g2", [128, D], F32)
        mv = modd.h.rearrange("(o j) -> o j", o=1)
        fw.dma(fw.sp, g2_b, g2_b[:], modd, mv[:, 5 * D:6 * D].partition_broadcast(128))
        xcs = [fw.sb(f"pd_xc{i}", [128, D], F32) for i in range(2)]
        for tt in range(NTL):
            xc = xcs[tt % 2]
            rows = slice(tt * 128, (tt + 1) * 128)
            fw.dma(fw.sp, xc, xc[:], x1d, x1d[rows, :])
            for dc in range(4):
                ot = Oacc[tt][dc]
                cs_ = slice(dc * 512, (dc + 1) * 512)
                fw.issue(fw.dve, lambda: nc.vector.tensor_tensor(out=ot[:], in0=ot[:], in1=g2_b[:, cs_], op=ALU.mult), reads=[ot, g2_b], writes=[ot])
                fw.issue(fw.pool, lambda: nc.gpsimd.tensor_tensor(out=xc[:, cs_], in0=xc[:, cs_], in1=ot[:], op=ALU.add), reads=[xc, ot], writes=[xc])
            fw.dma(fw.sp, xout, xout[rows, :], xc, xc[:])
        fw.barrier()
        fw.es = None


def emit_r3(fw, xsrc, mrg, idxm, modd, out_norm, w_out, norm_ffn, wq, k1, k2, u_tab, v_tab, cst, x1d, yd, xout, Gd, dbg=None, skip_apply=False):
    nc = fw.nc
    emit_wout(fw, xsrc, mrg, idxm, modd, out_norm, w_out, x1d)
    with ExitStack() as es:
        fw.es = es
        yT = fw.sb("r3_yT", [128, 16, NT], BF16)
        with ExitStack() as es2:
            fw.es = es2
            idb = make_identity(fw, "r3_idb", BF16)
            emit_norm_mod(fw, x1d, modd, 3 * D, 4 * D, norm_ffn, yT, idb, nt_tiles=NT // 128, ydst=None)
            fw.barrier()
        with ExitStack() as es3:
            fw.es = es3
            e1T = fw.sb("r3_e1T", [128, NT], F32)
            e2T = fw.sb("r3_e2T", [128, NT], F32)
            gT = fw.sb("r3_gT", [128, NT], F32)
            emit_peer_route(fw, yT, wq, k1, k2, cst, None, gT, dense=(e1T, e2T))
            fw.es = es3
            emit_peer_G(fw, e1T, e2T, gT, cst, Gd)
        fw.es = es
        if not skip_apply:
            emit_peer_dense(fw, yT, Gd, u_tab, v_tab, modd, x1d, xout)
        fw.es = None


B_ = 4
NCORES = 8
DEPTH = 2
_PROG = {}


def _host_consts():
    k = np.arange(128)[:, None]
    t = np.arange(128)[None, :]
    tri = (k <= t).astype(np.float32)
    sel = np.zeros((128, 128), np.float32)
    sel[127, :] = 1.0
    idn = np.eye(128, dtype=np.float32)
    c3 = np.ascontiguousarray(np.stack([tri, sel, idn], axis=1))
    dm = np.zeros((128, 16, 128), np.float32)
    for dl in range(16):
        delta = dl * 128 + t - k
        m = ((delta >= 0) & (delta <= 128)).astype(np.float32)
        m += ((delta >= 0) & (delta % 4 == 0) & (delta <= 512)).astype(np.float32)
        m += ((delta >= 0) & (delta % 16 == 0) & (delta <= 2048)).astype(np.float32)
        dm[:, dl, :] = m
    inv = np.power(10000.0, -np.arange(0, 64, 2, dtype=np.float32) / 64).astype(np.float32)
    ang = np.arange(2048, dtype=np.float32)[:, None] * inv[None, :]
    cs = np.concatenate([np.cos(ang), np.sin(ang)], axis=1).astype(np.float32)
    iota = np.ascontiguousarray(np.tile(np.arange(128, dtype=np.float32)[None, :], (128, 1)))
    return dict(c3=c3, dmask=dm, cs=cs, iota=iota)


_LAYER_IN = (("ada_w", [D, 3 * D]), ("ada_b", [3 * D]), ("norm_mix", [D]), ("w_in", [D, 6176]),
             ("fox_qn", [64]), ("fox_kn", [64]), ("dil_qn", [64]), ("dil_kn", [64]), ("fox_fb", [4]),
             ("mlstm_ib", [2]), ("mlstm_fb", [2]), ("convT", [512, 4]), ("out_norm", [D]), ("w_out", [D, D]),
             ("norm_ffn", [D]), ("wq", [D, D]), ("k1", [128, 128]), ("k2", [128, 128]),
             ("u_tab", [16384, D]), ("v_tab", [16384, D]))


_DBG = dict(depth=DEPTH, skip_apply=False, skip_mix=False)


def _build():
    depth = _DBG["depth"]
    nc = bass.Bass("TRN2", target_bir_lowering=False)
    fw = FW(nc)
    x = fw.dram("x", [NT, D], F32, kind="ExternalInput")
    cT = fw.dram("cT", [128, 16], F32, kind="ExternalInput")
    cst = {k: fw.dram(k, shp, F32, kind="ExternalInput") for k, shp in
           (("c3", [128, 3, 128]), ("dmask", [128, 16, 128]), ("cs", [S, 64]), ("iota", [128, 128]))}
    idxd = {k: fw.dram(k, shp, I32, kind="ExternalInput") for k, shp in
            (("idx_tok", [128, 32]), ("idx_feat", [128, 8]), ("idx_mrg", [128, 16]))}
    L = [{nm: fw.dram(f"{nm}_{l}", shp, F32, kind="ExternalInput") for nm, shp in _LAYER_IN} for l in range(DEPTH)]
    out = fw.dram("out", [NT, D], F32, kind="ExternalOutput")
    idx = {k: fw.sb("s_" + k, shp, I32) for k, shp in (("idx_tok", [128, 32]), ("idx_feat", [128, 8]), ("idx_mrg", [128, 16]))}
    for k in idx:
        fw.dma(fw.sp, idx[k], idx[k][:], idxd[k], idxd[k][:])
    xcur = x
    for l in range(depth):
        W = L[l]
        modh = fw.dram(f"modh_{l}", [3 * D], F32)
        modd = fw.dram(f"modd_{l}", [6 * D], F32)
        pj_tok = fw.dram(f"pj_tok_{l}", [NT, 5136], BF16)
        pj_feat = fw.dram(f"pj_feat_{l}", [1024, NT], BF16)
        pj_gate = fw.dram(f"pj_gate_{l}", [NT, 16], F32)
        pjt_all = fw.dram(f"pjt_all_{l}", [2 * NT, 5136], BF16)
        pjf_all = fw.dram(f"pjf_all_{l}", [2048, NT], BF16)
        pjg_all = fw.dram(f"pjg_all_{l}", [2 * NT, 16], F32)
        mg_src = fw.dram(f"mg_src_{l}", [S, 1024], BF16)
        mg_all = fw.dram(f"mg_all_{l}", [2 * S, 1024], BF16)
        x1d = fw.dram(f"x1d_{l}", [NT, D], F32)
        yd = None
        Gd = fw.dram(f"Gd_{l}", [128, 128, NT], BF16)
        xnext = out if l == depth - 1 else fw.dram(f"xmid_{l}", [NT, D], F32)
        emit_r1(fw, xcur, cT, W["ada_w"], W["ada_b"], W["norm_mix"], W["w_in"], modh, modd, pj_tok, pj_feat, pj_gate)
        allgather(fw, pj_tok, pjt_all, 128)
        allgather(fw, pj_feat, pjf_all, 512)
        allgather(fw, pj_gate, pjg_all, NT)
        tabs = dict(tok=alias(pjt_all, [4 * NT, 2568]), gate=alias(pjg_all, [4 * NT, 8]), feat=pjf_all,
                    idx=idx["idx_tok"], idxf=idx["idx_feat"])
        prm = {k: W[k] for k in ("fox_qn", "fox_kn", "dil_qn", "dil_kn", "fox_fb", "mlstm_ib", "mlstm_fb", "convT")}
        emit_mix_attn(fw, tabs, prm, cst, mg_src)
        emit_mix_mlstm(fw, tabs, prm, cst, mg_src)
        allgather(fw, mg_src, mg_all, 1024)
        emit_r3(fw, xcur, mg_all, idx["idx_mrg"], modd, W["out_norm"], W["w_out"], W["norm_ffn"], W["wq"], W["k1"], W["k2"],
                W["u_tab"], W["v_tab"], cst, x1d, yd, xnext, Gd, skip_apply=_DBG["skip_apply"])
        xcur = xnext
    fw.barrier()
    return nc


def _perm_w_in():
    cols = []
    for g in range(2):
        for base, w in ((0, 256), (512, 256), (1536, 256), (2048, 256), (1024, 256), (2560, 256)):
            cols += list(range(base + g * 256, base + g * 256 + w))
        cols += list(range(4096 + g * 512, 4096 + (g + 1) * 512))
        cols += list(range(5120 + g * 512, 5120 + (g + 1) * 512))
        cols += list(range(6144 + g * 4, 6144 + (g + 1) * 4))
        cols += list(range(6152 + g * 2, 6152 + (g + 1) * 2))
        cols += list(range(6156 + g * 2, 6156 + (g + 1) * 2))
    for g in range(2):
        cols += list(range(3072 + g * 256, 3072 + (g + 1) * 256))
        cols += list(range(3584 + g * 256, 3584 + (g + 1) * 256))
    for g in range(2):
        cols += list(range(6144 + g * 4, 6144 + (g + 1) * 4))
        cols += list(range(6152 + g * 2, 6152 + (g + 1) * 2))
        cols += list(range(6156 + g * 2, 6156 + (g + 1) * 2))
    assert len(cols) == 6176
    return np.asarray(cols)


def _perm_merged():
    rows = []
    for g in range(2):
        rows += list(range(g * 256, (g + 1) * 256))
        rows += list(range(512 + g * 256, 512 + (g + 1) * 256))
        rows += list(range(1024 + g * 512, 1024 + (g + 1) * 512))
    return np.asarray(rows)


def kernel(**inp):
    inp = {k: np.asarray(v) for k, v in inp.items()}
    hc = _host_consts()
    pw = _perm_w_in()
    pm = _perm_merged()
    shared = []
    for l in range(DEPTH):
        shared.append({
            f"norm_mix_{l}": np.ascontiguousarray(inp["norm_mix"][l]), f"w_in_{l}": np.ascontiguousarray(inp["w_in"][l][:, pw]),
            f"fox_qn_{l}": np.ascontiguousarray(inp["fox_qn"][l]), f"fox_kn_{l}": np.ascontiguousarray(inp["fox_kn"][l]),
            f"dil_qn_{l}": np.ascontiguousarray(inp["dil_qn"][l]), f"dil_kn_{l}": np.ascontiguousarray(inp["dil_kn"][l]),
            f"out_norm_{l}": np.ascontiguousarray(inp["out_norm"][l][pm]), f"w_out_{l}": np.ascontiguousarray(inp["w_out"][l][pm, :]),
            f"norm_ffn_{l}": np.ascontiguousarray(inp["norm_ffn"][l]), f"wq_{l}": np.ascontiguousarray(inp["peer_wq"][l]),
            f"k1_{l}": np.ascontiguousarray(inp["peer_k1"][l]), f"k2_{l}": np.ascontiguousarray(inp["peer_k2"][l]),
            f"u_tab_{l}": np.ascontiguousarray(inp["peer_u"][l]), f"v_tab_{l}": np.ascontiguousarray(inp["peer_v"][l]),
        })
    p = np.arange(128, dtype=np.int64)[:, None]
    adah = [[(np.ascontiguousarray(inp["ada_w"][l][:, g * 3 * D:(g + 1) * 3 * D]), np.ascontiguousarray(inp["ada_b"][l][g * 3 * D:(g + 1) * 3 * D]))
             for g in range(2)] for l in range(DEPTH)]
    in_maps = []
    for c in range(NCORES):
        b, g = c // 2, c % 2
        m = {"x": np.ascontiguousarray(inp["x"][b, g * NT:(g + 1) * NT]),
             "cT": np.ascontiguousarray(inp["c"][b].reshape(16, 128).T),
             "c3": hc["c3"], "dmask": hc["dmask"], "cs": hc["cs"], "iota": hc["iota"]}
        i16 = np.arange(16, dtype=np.int64)[None, :]
        Tk = i16 * 128 + p
        r_, t_ = Tk // NT, Tk % NT
        it = ((((t_ // 128) * 2 + r_) * 128 + t_ % 128) * 2 + g)
        ig_ = (r_ * NT + t_) * 2 + g
        m["idx_tok"] = np.ascontiguousarray(np.concatenate([it, ig_], axis=1).astype(np.int32))
        rm = np.arange(8, dtype=np.int64)[None, :]
        m["idx_feat"] = np.ascontiguousarray(((g * 2 + rm // 4) * 512 + (rm % 4) * 128 + p).astype(np.int32))
        Tm = g * NT + (i16 % 8) * 128 + p
        m["idx_mrg"] = np.ascontiguousarray((((Tm // 1024) * 2 + i16 // 8) * 1024 + Tm % 1024).astype(np.int32))
        for l in range(DEPTH):
            m.update(shared[l])
            convT = inp["mlstm_conv"][l].T
            m[f"convT_{l}"] = np.ascontiguousarray(np.concatenate([convT[g * 256:(g + 1) * 256], convT[512 + g * 256:512 + (g + 1) * 256]], axis=0))
            m[f"fox_fb_{l}"] = np.ascontiguousarray(inp["fox_fb"][l][g * 4:(g + 1) * 4])
            m[f"ada_w_{l}"] = adah[l][g][0]
            m[f"ada_b_{l}"] = adah[l][g][1]
            m[f"mlstm_ib_{l}"] = np.ascontiguousarray(inp["mlstm_ib"][l][g * 2:(g + 1) * 2])
            m[f"mlstm_fb_{l}"] = np.ascontiguousarray(inp["mlstm_fb"][l][g * 2:(g + 1) * 2])
        in_maps.append(m)
    if "nc" not in _PROG:
        _PROG["nc"] = _build()
    res = run_bass_kernel_spmd(_PROG["nc"], in_maps, core_ids=list(range(NCORES)))
    out = np.empty((B_, S, D), np.float32)
    for c in range(NCORES):
        out[c // 2, (c % 2) * NT:(c % 2 + 1) * NT] = res.results[c]["out"]
    return out
```

```python
from contextlib import ExitStack
import numpy as np
import concourse.bass as bass
import concourse.mybir as mybir
from concourse.bass_utils import run_bass_kernel_spmd

F32 = mybir.dt.float32
BF16 = mybir.dt.bfloat16
I32 = mybir.dt.int32
U32 = mybir.dt.uint32
AF = mybir.ActivationFunctionType
ALU = mybir.AluOpType
AX = mybir.AxisListType

D = 2048
DIN = 6160
S = 2048
NT = 1024
EPS = 1e-6


class T:
    __slots__ = ("h", "w", "r", "name")

    def __init__(self, h, name=""):
        self.h = h
        self.w = {}
        self.r = {}
        self.name = name

    def __getitem__(self, idx):
        return self.h[idx]


class Eng:
    def __init__(self, fw, name, obj, nsem=1, inc=1, is_dma=None):
        self.name = name
        self.obj = obj
        self.inc = inc
        self.sems = [fw.nc.alloc_semaphore(f"s_{name}_{i}") for i in range(nsem)]
        self.cnt = [0] * nsem
        self.n = 0
        self.seen = {}
        self.is_dma = (inc == 16) if is_dma is None else is_dma


class FW:
    def __init__(self, nc, ndma_sems=8):
        self.nc = nc
        self.semtab = {}
        self.pe = Eng(self, "pe", nc.tensor)
        self.act = Eng(self, "act", nc.scalar)
        self.dve = Eng(self, "dve", nc.vector)
        self.pool = Eng(self, "pool", nc.gpsimd)
        self.sp = Eng(self, "sp", nc.sync, nsem=ndma_sems, inc=16)
        self.gq = Eng(self, "gq", nc.gpsimd, nsem=ndma_sems, inc=16)
        self.aq = Eng(self, "aq", nc.scalar, nsem=ndma_sems, inc=16)
        self.cc = Eng(self, "cc", nc.gpsimd, nsem=1, inc=1, is_dma=True)
        self.engs = (self.pe, self.act, self.dve, self.pool, self.sp, self.gq, self.aq, self.cc)
        self.stream = {"pe": self.pe, "act": self.act, "dve": self.dve, "pool": self.pool,
                       "sp": self.sp, "gq": self.pool, "aq": self.act, "cc": self.pool}
        for e in self.engs:
            for i, s in enumerate(e.sems):
                self.semtab[(e.name, i)] = s
        self.nwaits = 0
        self.ninst = 0
        self.es = None

    def sb(self, name, shape, dt):
        self.uid = getattr(self, "uid", 0) + 1
        name = f"{name}_{self.uid}"
        if self.es is not None:
            return T(self.es.enter_context(self.nc.sbuf_tensor(name, list(shape), dt)), name)
        return T(self.nc.alloc_sbuf_tensor(name, list(shape), dt), name)

    def ps(self, name, shape, dt=F32):
        self.uid = getattr(self, "uid", 0) + 1
        name = f"{name}_{self.uid}"
        if self.es is not None:
            return T(self.es.enter_context(self.nc.psum_tensor(name, list(shape), dt)), name)
        return T(self.nc.alloc_psum_tensor(name, list(shape), dt), name)

    def dram(self, name, shape, dt, kind="Internal"):
        return T(self.nc.dram_tensor(name, list(shape), dt, kind=kind), name)

    def _wait(self, eng, dep):
        if dep is None:
            return
        key, val = dep
        st = self.stream[eng.name]
        if st.seen.get(key, 0) >= val:
            return
        st.seen[key] = val
        st.obj.wait_ge(self.semtab[key], val)
        self.nwaits += 1

    def issue(self, eng, fn, reads=(), writes=()):
        if eng.is_dma:
            slot = eng.n % len(eng.sems)
            if eng.cnt[slot] > 0:
                self._wait(eng, ((eng.name, slot), eng.cnt[slot]))
        else:
            slot = 0
        mine = eng.name
        for t in reads:
            for k, v in t.w.items():
                if k[0] == mine and eng is self.pe:
                    continue
                self._wait(eng, (k, v))
        for t in writes:
            for k, v in t.w.items():
                if k[0] == mine and not eng.is_dma:
                    continue
                self._wait(eng, (k, v))
            for k, v in t.r.items():
                if k[0] == mine and not eng.is_dma:
                    continue
                self._wait(eng, (k, v))
        ins = fn()
        eng.n += 1
        eng.cnt[slot] += eng.inc
        ins.then_inc(eng.sems[slot], eng.inc)
        key = (eng.name, slot)
        val = eng.cnt[slot]
        for t in reads:
            if t.r.get(key, 0) < val:
                t.r[key] = val
        for t in writes:
            t.w[key] = val
            t.r.clear()
        self.ninst += 1
        return ins

    def barrier(self):
        for sname in ("pe", "act", "dve", "pool", "sp"):
            st = self.stream[sname]
            for e in self.engs:
                for i in range(len(e.sems)):
                    if e.cnt[i] > 0:
                        self._wait(st, ((e.name, i), e.cnt[i]))

    def dma(self, q, out_t, out_ap, in_t, in_ap, **kw):
        return self.issue(q, lambda: q.obj.dma_start(out=out_ap, in_=in_ap, **kw), reads=[in_t], writes=[out_t])


def alias(t, new_shape):
    v = T(t.h.reshape(list(new_shape)), t.name + "_v")
    v.w = t.w
    v.r = t.r
    return v


def make_identity(fw, name, dt):
    nc = fw.nc
    idn = fw.sb(name, [128, 128], dt)
    fw.issue(fw.pool, lambda: nc.gpsimd.memset(idn[:], 1.0), writes=[idn])
    fw.issue(fw.pool, lambda: nc.gpsimd.affine_select(out=idn[:], in_=idn[:], pattern=[[-1, 128]],
                                                      compare_op=ALU.is_equal, fill=0.0, base=0,
                                                      channel_multiplier=1), reads=[idn], writes=[idn])
    return idn


def bcast_row(ap_row, nparts=128):
    return ap_row.partition_broadcast(nparts)


def rstd_from_ss(fw, s_, inv_n, c0=0, c1=1, w=1):
    nc = fw.nc
    fw.issue(fw.dve, lambda: nc.vector.tensor_scalar(out=s_[:, c1:c1 + w], in0=s_[:, c0:c0 + w], scalar1=inv_n, scalar2=EPS,
                                                     op0=ALU.mult, op1=ALU.add), reads=[s_], writes=[s_])
    fw.issue(fw.act, lambda: nc.scalar.activation(out=s_[:, c1:c1 + w], in_=s_[:, c1:c1 + w], func=AF.Ln), reads=[s_], writes=[s_])
    fw.issue(fw.act, lambda: nc.scalar.activation(out=s_[:, c1:c1 + w], in_=s_[:, c1:c1 + w], func=AF.Exp, scale=-0.5), reads=[s_], writes=[s_])

def emit_mod(fw, cT, ada_w, ada_b, modd):
    nc = fw.nc
    with ExitStack() as es:
        fw.es = es
        cs = fw.sb("mod_c", [128, 16], F32)
        cb = fw.sb("mod_cb", [128, 16], BF16)
        wts = [fw.sb(f"mod_w{i}", [128, 16, 512], BF16) for i in range(2)]
        bts = [fw.sb(f"mod_b{i}", [1, 512], F32) for i in range(2)]
        sts = [fw.sb(f"mod_s{i}", [1, 512], F32) for i in range(2)]
        pss = [fw.ps(f"mod_p{i}", [128, 512], F32) for i in range(2)]
        fw.dma(fw.sp, cs, cs[:], cT, cT[:])
        fw.issue(fw.act, lambda: nc.scalar.activation(out=cb[:], in_=cs[:], func=AF.Silu), reads=[cs], writes=[cb])
        wv = ada_w.h.rearrange("(k p) j -> p k j", p=128)
        bv = ada_b.h.rearrange("(o j) -> o j", o=1)
        mv = modd.h.rearrange("(o j) -> o j", o=1)
        for jc in range(12):
            w = wts[jc % 2]; b = bts[jc % 2]; st = sts[jc % 2]; p = pss[jc % 2]
            js = slice(jc * 512, (jc + 1) * 512)
            fw.dma(fw.gq, w, w[:], ada_w, wv[:, :, js])
            fw.dma(fw.sp, b, b[:], ada_b, bv[:, js])
            for k in range(16):
                fw.issue(fw.pe, lambda k=k: nc.tensor.matmul(p[0:1, :], lhsT=cb[:, k:k + 1], rhs=w[:, k, :],
                                                             start=(k == 0), stop=(k == 15)),
                         reads=[cb, w], writes=[p])
            fw.issue(fw.dve, lambda: nc.vector.tensor_tensor(out=st[:], in0=p[0:1, :], in1=b[:], op=ALU.add),
                     reads=[p, b], writes=[st])
            fw.dma(fw.sp, modd, mv[:, js], st, st[:])
        fw.barrier()
        fw.es = None


def emit_norm_mod(fw, xsrc, modd, off_shift, off_scale, gain, hT, idb, nt_tiles=8, ydst=None):
    nc = fw.nc
    es = fw.es
    nm_b = fw.sb("nm_b", [128, D], F32)
    sc_b = fw.sb("sc_b", [128, D], F32)
    sh_b = fw.sb("sh_b", [128, D], F32)
    xts = [fw.sb(f"nm_x{i}", [128, D], F32) for i in range(3)]
    hfs = [fw.sb(f"nm_hf{i}", [128, D], F32) for i in range(2)]
    hbs = [fw.sb(f"nm_hb{i}", [128, D], BF16) for i in range(3)]
    junk = fw.sb("nm_junk", [128, D], BF16)
    ss = [fw.sb(f"nm_ss{i}", [128, 2], F32) for i in range(3)]
    trs = [fw.ps(f"nm_tr{i}", [128, 1024], BF16) for i in range(2)]
    mv = modd.h.rearrange("(o j) -> o j", o=1)
    gv = gain.h.rearrange("(o j) -> o j", o=1)
    fw.dma(fw.sp, nm_b, nm_b[:], gain, gv[:, :].partition_broadcast(128))
    fw.dma(fw.sp, sc_b, sc_b[:], modd, mv[:, off_scale:off_scale + D].partition_broadcast(128))
    fw.dma(fw.sp, sh_b, sh_b[:], modd, mv[:, off_shift:off_shift + D].partition_broadcast(128))
    fw.issue(fw.dve, lambda: nc.vector.scalar_tensor_tensor(out=sc_b[:], in0=sc_b[:], scalar=1.0, in1=nm_b[:],
                                                            op0=ALU.add, op1=ALU.mult),
             reads=[sc_b, nm_b], writes=[sc_b])
    for i in range(nt_tiles):
        xt = xts[i % 3]; hb = hbs[i % 3]; s_ = ss[i % 3]; hf = hfs[i % 2]
        fw.dma(fw.sp, xt, xt[:], xsrc, xsrc[i * 128:(i + 1) * 128, :])
        fw.issue(fw.act, lambda: nc.scalar.activation(out=junk[:], in_=xt[:], func=AF.Square, accum_out=s_[:, 0:1]),
                 reads=[xt], writes=[junk, s_])
        rstd_from_ss(fw, s_, 1.0 / D)
        fw.issue(fw.dve, lambda: nc.vector.scalar_tensor_tensor(out=hf[:], in0=xt[:], scalar=s_[:, 1:2], in1=sc_b[:],
                                                                op0=ALU.mult, op1=ALU.mult),
                 reads=[xt, s_, sc_b], writes=[hf])
        eng_a = fw.pool if i % 2 == 0 else fw.dve
        fw.issue(eng_a, lambda: eng_a.obj.tensor_tensor(out=hb[:], in0=hf[:], in1=sh_b[:], op=ALU.add),
                 reads=[hf, sh_b], writes=[hb])
        if ydst is not None:
            fw.dma(fw.aq, ydst, ydst[i * 128:(i + 1) * 128, :], hb, hb[:])
        for half in range(2):
            tr = trs[half]
            for kk in range(8):
                k = half * 8 + kk
                fw.issue(fw.pe, lambda k=k, kk=kk, tr=tr: nc.tensor.transpose(tr[:, kk * 128:(kk + 1) * 128],
                                                                             hb[:, k * 128:(k + 1) * 128], idb[:]),
                         reads=[hb, idb], writes=[tr])
            dst = hT[:, half * 8:(half + 1) * 8, i * 128:(i + 1) * 128]
            src = tr[:].rearrange("p (k t) -> p k t", k=8)
            if half == 0:
                fw.issue(fw.act, lambda dst=dst, src=src: nc.scalar.copy(out=dst, in_=src), reads=[tr], writes=[hT])
            else:
                fw.issue(fw.dve, lambda dst=dst, src=src: nc.vector.tensor_copy(out=dst, in_=src), reads=[tr], writes=[hT])


def emit_r1(fw, xsrc, cT, ada_w, ada_b, norm_mix, w_in, modh, modd, pj_tok, pj_feat, pj_gate):
    nc = fw.nc
    emit_mod(fw, cT, ada_w, ada_b, modh)
    allgather(fw, alias(modh, [48, 128]), alias(modd, [96, 128]), 48)
    with ExitStack() as es:
        fw.es = es
        idb = make_identity(fw, "r1_idb", BF16)
        hT = fw.sb("r1_hT", [128, 16, NT], BF16)
        with ExitStack() as es2:
            fw.es = es2
            emit_norm_mod(fw, xsrc, modd, 0, D, norm_mix, hT, idb, nt_tiles=NT // 128)
            fw.barrier()
        fw.es = es
        wts = [fw.sb(f"r1_w{i}", [128, 16, 512], BF16) for i in range(2)]
        stb = [fw.sb(f"r1_st{i}", [128, 512], BF16) for i in range(4)]
        stf = [fw.sb(f"r1_sf{i}", [128, 512], F32) for i in range(4)]
        pss = [fw.ps(f"r1_p{i}", [128, 512], F32) for i in range(4)]
        wv = w_in.h.rearrange("(k p) j -> p k j", p=128)
        plan = [("tok", n * 512, 512, n * 512) for n in range(10)] + [("tok", 5120, 16, 5120)]
        plan += [("feat", 5136, 512, 0), ("feat", 5648, 512, 512), ("gate", 6160, 16, 0)]
        cnt = 0
        for n, (kind, c0, ncol, d0) in enumerate(plan):
            w = wts[n % 2]
            sts = stf if kind == "gate" else stb
            fw.dma(fw.gq, w, w[:, :, 0:ncol], w_in, wv[:, :, c0:c0 + ncol])
            if kind == "feat":
                for fc in range(4):
                    for th in range(NT // 512):
                        p = pss[cnt % 4]; st = sts[cnt % 4]
                        for k in range(16):
                            fw.issue(fw.pe, lambda: nc.tensor.matmul(
                                p[:, :], lhsT=w[:, k, fc * 128:(fc + 1) * 128], rhs=hT[:, k, th * 512:(th + 1) * 512],
                                start=(k == 0), stop=(k == 15)), reads=[w, hT], writes=[p])
                        if cnt % 2 == 0:
                            fw.issue(fw.act, lambda: nc.scalar.copy(out=st[:], in_=p[:]), reads=[p], writes=[st])
                        else:
                            fw.issue(fw.dve, lambda: nc.vector.tensor_copy(out=st[:], in_=p[:]), reads=[p], writes=[st])
                        r0 = d0 + fc * 128
                        fw.dma(fw.sp, pj_feat, pj_feat[r0:r0 + 128, th * 512:(th + 1) * 512], st, st[:])
                        cnt += 1
            else:
                dst = pj_tok if kind == "tok" else pj_gate
                for i in range(NT // 128):
                    p = pss[cnt % 4]; st = sts[cnt % 4]
                    for k in range(16):
                        fw.issue(fw.pe, lambda: nc.tensor.matmul(
                            p[:, 0:ncol], lhsT=hT[:, k, i * 128:(i + 1) * 128], rhs=w[:, k, 0:ncol],
                            start=(k == 0), stop=(k == 15)), reads=[w, hT], writes=[p])
                    if cnt % 2 == 0:
                        fw.issue(fw.act, lambda: nc.scalar.copy(out=st[:, 0:ncol], in_=p[:, 0:ncol]), reads=[p], writes=[st])
                    else:
                        fw.issue(fw.dve, lambda: nc.vector.tensor_copy(out=st[:, 0:ncol], in_=p[:, 0:ncol]), reads=[p], writes=[st])
                    fw.dma(fw.sp, dst, dst[i * 128:(i + 1) * 128, d0:d0 + ncol], st, st[:, 0:ncol])
                    cnt += 1
        fw.barrier()
        fw.es = None


def allgather(fw, src, dst, rpc):
    nc = fw.nc
    rg = [[0, 1], [2, 3], [4, 5], [6, 7]]
    R = src.h.shape[0]
    assert R % rpc == 0
    fw.barrier()
    for k in range(R // rpc):
        fw.issue(fw.cc, lambda: nc.gpsimd.collective_compute("AllGather", ALU.bypass, replica_groups=rg,
                                                             ins=[src.h[k * rpc:(k + 1) * rpc, :]],
                                                             outs=[dst.h[k * 2 * rpc:(k + 1) * 2 * rpc, :]]),
                 reads=[src], writes=[dst])
    fw.barrier()


def gather_rows(fw, out_t, out_ap, table, idx_t, col):
    nc = fw.nc
    return fw.issue(fw.gq, lambda: nc.gpsimd.indirect_dma_start(out=out_ap, out_offset=None, in_=table.h[:, :],
                                                                in_offset=bass.IndirectOffsetOnAxis(ap=idx_t[:, col:col + 1], axis=0)),
                    reads=[table, idx_t], writes=[out_t])


def emit_mix_attn(fw, tabs, prm, cst, merged):
    nc = fw.nc
    NTL = S // 128
    with ExitStack() as es:
        fw.es = es
        qkT = fw.sb("ma_qkT", [128, 8, S], BF16)
        Vaug = fw.sb("ma_V", [128, NTL, 8, 65], BF16)
        mk = fw.sb("ma_mk", [128, 16, 128], BF16)
        cm = fw.sb("ma_cm", [128, 128], BF16)
        BT = fw.sb("ma_BT", [128, 4, 16, 16], F32)
        c3 = fw.sb("ma_c3", [128, 3, 128], F32)
        ones = fw.sb("ma_ones", [128, 128], F32)
        fw.dma(fw.sp, c3, c3[:], cst["c3"], cst["c3"][:])
        fw.issue(fw.pool, lambda: nc.gpsimd.memset(ones[:], 1.0), writes=[ones])
        fw.issue(fw.pool, lambda: nc.gpsimd.memset(Vaug[:, :, :, 64:65], 1.0), writes=[Vaug])
        fw.issue(fw.dve, lambda: nc.vector.tensor_copy(out=cm[:], in_=c3[:, 0, :]), reads=[c3], writes=[cm])
        with ExitStack() as es1:
            fw.es = es1
            idb = make_identity(fw, "ma_idb", BF16)
            mstage = fw.sb("ma_mst", [128, 16, 128], F32)
            fw.dma(fw.sp, mstage, mstage[:], cst["dmask"], cst["dmask"][:])
            fw.issue(fw.dve, lambda: nc.vector.tensor_copy(out=mk[:], in_=mstage[:]), reads=[mstage], writes=[mk])
            G4 = fw.sb("ma_G4", [128, 4, 64], F32)
            for w, nm in enumerate(("fox_qn", "fox_kn", "dil_qn", "dil_kn")):
                gv = prm[nm].h.rearrange("(o j) -> o j", o=1)
                fw.dma(fw.sp, G4, G4[:, w, :], prm[nm], gv[:, :].partition_broadcast(128))
            for w in (0, 2):
                fw.issue(fw.dve, lambda: nc.vector.tensor_scalar_mul(out=G4[:, w, :], in0=G4[:, w, :], scalar1=0.125),
                         reads=[G4], writes=[G4])
            fb = fw.sb("ma_fb", [128, 4], F32)
            fbv = prm["fox_fb"].h.rearrange("(o j) -> o j", o=1)
            fw.dma(fw.sp, fb, fb[:], prm["fox_fb"], fbv[:, :].partition_broadcast(128))
            ffT = fw.sb("ma_ffT", [128, NTL, 4], F32)
            gts = fw.sb("ma_gts", [128, NTL, 8], F32)
            for i in range(NTL):
                gather_rows(fw, gts, gts[:, i, :], tabs["gate"], tabs["idx"], 16 + i)
            fw.issue(fw.dve, lambda: nc.vector.tensor_tensor(out=ffT[:], in0=gts[:, :, 0:4], in1=fb[:].unsqueeze(1).to_broadcast([128, NTL, 4]),
                                                             op=ALU.add), reads=[gts, fb], writes=[ffT])
            fw.issue(fw.act, lambda: nc.scalar.activation(out=ffT[:], in_=ffT[:], func=AF.Exp, scale=-1.0), reads=[ffT], writes=[ffT])
            fw.issue(fw.act, lambda: nc.scalar.activation(out=ffT[:], in_=ffT[:], func=AF.Ln, bias=1.0), reads=[ffT], writes=[ffT])
            pF1 = fw.ps("ma_pF1", [128, 512], F32)
            pF2 = fw.ps("ma_pF2", [128, 512], F32)
            lf2 = ffT[:].rearrange("p i c -> p (i c)")
            fw.issue(fw.pe, lambda: nc.tensor.matmul(pF1[:, 0:64], lhsT=c3[:, 0, :], rhs=lf2, start=True, stop=True),
                     reads=[c3, ffT], writes=[pF1])
            fw.issue(fw.pe, lambda: nc.tensor.matmul(pF2[:, 0:64], lhsT=ones[:], rhs=lf2, start=True, stop=True),
                     reads=[ones, ffT], writes=[pF2])
            tot = fw.sb("ma_tot", [128, NTL, 4], F32)
            offs = fw.sb("ma_offs", [128, NTL, 4], F32)
            Fp = fw.sb("ma_Fp", [128, NTL, 4], F32)
            FR = fw.sb("ma_FR", [128, NTL, 4], F32)
            fw.issue(fw.dve, lambda: nc.vector.tensor_copy(out=tot[:].rearrange("p i c -> p (i c)"), in_=pF2[:, 0:64]),
                     reads=[pF2], writes=[tot])
            fw.issue(fw.dve, lambda: nc.vector.memset(offs[:, 0, :], 0.0), writes=[offs])
            for j in range(1, NTL):
                fw.issue(fw.dve, lambda: nc.vector.tensor_tensor(out=offs[:, j, :], in0=offs[:, j - 1, :], in1=tot[:, j - 1, :], op=ALU.add),
                         reads=[offs, tot], writes=[offs])
            fw.issue(fw.dve, lambda: nc.vector.tensor_tensor(out=Fp[:].rearrange("p i c -> p (i c)"), in0=pF1[:, 0:64],
                                                             in1=offs[:].rearrange("p i c -> p (i c)"), op=ALU.add),
                     reads=[pF1, offs], writes=[Fp])
            fw.issue(fw.pe, lambda: nc.tensor.matmul(pF2[:, 0:64], lhsT=c3[:, 1, :], rhs=Fp[:].rearrange("p i c -> p (i c)"),
                                                     start=True, stop=True), reads=[c3, Fp], writes=[pF2])
            fw.issue(fw.dve, lambda: nc.vector.tensor_copy(out=FR[:].rearrange("p i c -> p (i c)"), in_=pF2[:, 0:64]),
                     reads=[pF2], writes=[FR])
            for i in range(NTL):
                fw.issue(fw.dve, lambda: nc.vector.tensor_tensor(
                    out=BT[:, :, i, :], in0=Fp[:].rearrange("p j h -> p h j"),
                    in1=FR[:, i, :].unsqueeze(2).to_broadcast([128, 4, NTL]), op=ALU.subtract),
                    reads=[Fp, FR], writes=[BT])
            RTs = [fw.sb(f"ma_RT{i}", [128, 2568], BF16) for i in range(4)]
            css = [fw.sb(f"ma_cs{i}", [128, 64], F32) for i in range(2)]
            tmp = fw.sb("ma_tmp", [128, 1024], F32)
            QKn = fw.sb("ma_QKn", [128, 1024], F32)
            QKb = fw.sb("ma_QKb", [128, 1024], BF16)
            rA = fw.sb("ma_rA", [128, 8, 32], F32)
            rB = fw.sb("ma_rB", [128, 8, 32], F32)
            rC = fw.sb("ma_rC", [128, 8, 32], F32)
            rD = fw.sb("ma_rD", [128, 8, 32], F32)
            st = fw.sb("ma_st", [128, 32], F32)
            trp = [fw.ps(f"ma_tr{i}", [128, 1024], BF16) for i in range(2)]
            for i in range(NTL):
                RT = RTs[i % 4]; cs = css[i % 2]; tr = trp[i % 2]
                rows = slice(i * 128, (i + 1) * 128)
                if i == 0:
                    for i2 in range(3):
                        gather_rows(fw, RTs[i2 % 4], RTs[i2 % 4][:, :], tabs["tok"], tabs["idx"], i2)
                if i + 3 < NTL:
                    gather_rows(fw, RTs[(i + 3) % 4], RTs[(i + 3) % 4][:, :], tabs["tok"], tabs["idx"], i + 3)
                fw.dma(fw.sp, cs, cs[:], cst["cs"], cst["cs"][rows, :])
                fw.issue(fw.act, lambda: nc.scalar.activation(out=tmp[:], in_=RT[:, 0:1024], func=AF.Square), reads=[RT], writes=[tmp])
                fw.issue(fw.dve, lambda: nc.vector.tensor_reduce(out=st[:, 0:16], in_=tmp[:].rearrange("p (g d) -> p g d", d=64),
                                                                 axis=AX.X, op=ALU.add), reads=[tmp], writes=[st])
                rstd_from_ss(fw, st, 1.0 / 64, c0=0, c1=16, w=16)
                fw.issue(fw.dve, lambda: nc.vector.tensor_tensor(
                    out=QKn[:].rearrange("p (g d) -> p g d", d=64), in0=RT[:, 0:1024].rearrange("p (g d) -> p g d", d=64),
                    in1=st[:, 16:32].unsqueeze(2).to_broadcast([128, 16, 64]), op=ALU.mult), reads=[RT, st], writes=[QKn])
                fw.issue(fw.pool, lambda: nc.gpsimd.tensor_tensor(
                    out=QKb[:, 0:512].rearrange("p (w h d) -> p w h d", w=2, d=64),
                    in0=QKn[:, 0:512].rearrange("p (w h d) -> p w h d", w=2, d=64),
                    in1=G4[:, 0:2, :].unsqueeze(2).to_broadcast([128, 2, 4, 64]), op=ALU.mult), reads=[QKn, G4], writes=[QKb])
                fw.issue(fw.dve, lambda: nc.vector.tensor_tensor(
                    out=QKn[:, 512:1024].rearrange("p (w h d) -> p w h d", w=2, d=64),
                    in0=QKn[:, 512:1024].rearrange("p (w h d) -> p w h d", w=2, d=64),
                    in1=G4[:, 2:4, :].unsqueeze(2).to_broadcast([128, 2, 4, 64]), op=ALU.mult), reads=[QKn, G4], writes=[QKn])
                dn = QKn[:, 512:1024].rearrange("p (g two d) -> p g two d", two=2, d=32)
                db = QKb[:, 512:1024].rearrange("p (g two d) -> p g two d", two=2, d=32)
                cosb = cs[:, 0:32].unsqueeze(1).to_broadcast([128, 8, 32])
                sinb = cs[:, 32:64].unsqueeze(1).to_broadcast([128, 8, 32])
                fw.issue(fw.dve, lambda: nc.vector.tensor_tensor(out=rA[:], in0=dn[:, :, 0, :], in1=cosb, op=ALU.mult), reads=[QKn, cs], writes=[rA])
                fw.issue(fw.pool, lambda: nc.gpsimd.tensor_tensor(out=rB[:], in0=dn[:, :, 1, :], in1=sinb, op=ALU.mult), reads=[QKn, cs], writes=[rB])
                fw.issue(fw.dve, lambda: nc.vector.tensor_tensor(out=rC[:], in0=dn[:, :, 1, :], in1=cosb, op=ALU.mult), reads=[QKn, cs], writes=[rC])
                fw.issue(fw.pool, lambda: nc.gpsimd.tensor_tensor(out=rD[:], in0=dn[:, :, 0, :], in1=sinb, op=ALU.mult), reads=[QKn, cs], writes=[rD])
                fw.issue(fw.dve, lambda: nc.vector.tensor_tensor(out=db[:, :, 0, :], in0=rA[:], in1=rB[:], op=ALU.subtract), reads=[rA, rB], writes=[QKb])
                fw.issue(fw.dve, lambda: nc.vector.tensor_tensor(out=db[:, :, 1, :], in0=rC[:], in1=rD[:], op=ALU.add), reads=[rC, rD], writes=[QKb])
                for m in range(8):
                    fw.issue(fw.pe, lambda: nc.tensor.transpose(tr[:, m * 128:(m + 1) * 128], QKb[:, m * 128:(m + 1) * 128], idb[:]),
                             reads=[QKb, idb], writes=[tr])
                fw.issue(fw.act, lambda: nc.scalar.copy(out=qkT[:, :, rows], in_=tr[:].rearrange("p (m t) -> p m t", m=8)),
                         reads=[tr], writes=[qkT])
                fw.issue(fw.pool, lambda: nc.gpsimd.tensor_copy(out=Vaug[:, i, :, 0:64], in_=RT[:, 1024:1536].rearrange("p (h d) -> p h d", d=64)),
                         reads=[RT], writes=[Vaug])
            fw.barrier()
        fw.es = es
        pS = [fw.ps(f"ma_pS{i}", [128, 512], F32) for i in range(3)]
        pO = [fw.ps(f"ma_pO{i}", [128, 128], F32) for i in range(4)]
        NPT = 8
        PTs = [fw.sb(f"ma_PT{i}", [128, 128], BF16) for i in range(NPT)]
        PMs = [fw.sb(f"ma_PM{i}", [128, 128], BF16) for i in range(NPT)]
        osb = [fw.sb(f"ma_o{i}", [128, 4, 64], F32) for i in range(2)]
        onb = [fw.sb(f"ma_on{i}", [128, 4, 64], BF16) for i in range(2)]
        sq = fw.sb("ma_sq", [128, 256], F32)
        est = [fw.sb(f"ma_est{i}", [128, 16], F32) for i in range(2)]
        steps = []
        for I in range(NTL // 4):
            for hh in range(8):
                for j in range(4 * I + 4):
                    steps.append((I, hh, j))
        nsteps = len(steps)
        cnt = dict(m=0)

        def stage_s(n):
            I, hh, j = steps[n]
            typ, h = hh // 4, hh % 4
            hp, hl = h // 2, h % 2
            qsel = (0 if typ == 0 else 2) * 2 + hp
            ksel = (1 if typ == 0 else 3) * 2 + hp
            pr = slice(hl * 64, hl * 64 + 64)
            ps = pS[n % 3]
            fw.issue(fw.pe, lambda: nc.tensor.matmul(ps[:], lhsT=qkT[pr, ksel, j * 128:(j + 1) * 128],
                                                     rhs=qkT[pr, qsel, I * 512:(I + 1) * 512], start=True, stop=True),
                     reads=[qkT], writes=[ps])

        def epilogue(u, I, hh):
            o = osb[u % 2]; on = onb[u % 2]; e = est[u % 2]
            for il in range(4):
                po = pO[il]
                fw.issue(fw.dve, lambda: nc.vector.reciprocal(out=e[:, il:il + 1], in_=po[:, 64:65]), reads=[po], writes=[e])
                fw.issue(fw.dve, lambda: nc.vector.tensor_scalar_mul(out=o[:, il, :], in0=po[:, 0:64], scalar1=e[:, il:il + 1]), reads=[po, e], writes=[o])
            fw.issue(fw.act, lambda: nc.scalar.activation(out=sq[:], in_=o[:].rearrange("p h d -> p (h d)"), func=AF.Square), reads=[o], writes=[sq])
            fw.issue(fw.dve, lambda: nc.vector.tensor_reduce(out=e[:, 4:8], in_=sq[:].rearrange("p (h d) -> p h d", d=64), axis=AX.X, op=ALU.add),
                     reads=[sq], writes=[e])
            rstd_from_ss(fw, e, 1.0 / 64, c0=4, c1=8, w=4)
            fw.issue(fw.dve, lambda: nc.vector.tensor_tensor(out=on[:], in0=o[:], in1=e[:, 8:12].unsqueeze(2).to_broadcast([128, 4, 64]), op=ALU.mult),
                     reads=[o, e], writes=[on])
            fw.dma(fw.sp, merged, merged.h[I * 512:(I + 1) * 512, hh * 64:(hh + 1) * 64].rearrange("(i p) d -> p i d", p=128), on, on[:])

        def stage_e(n):
            I, hh, j = steps[n]
            typ, h = hh // 4, hh % 4
            u = I * 8 + hh
            ps = pS[n % 3]
            todo = []
            for il in range(4):
                i = 4 * I + il
                if i < j:
                    continue
                m = cnt["m"]; cnt["m"] += 1
                PT = PTs[m % NPT]; PM = PMs[m % NPT]
                src = ps[:, il * 128:(il + 1) * 128]
                if typ == 0:
                    fw.issue(fw.act, lambda: nc.scalar.activation(out=PT[:], in_=src, func=AF.Exp, bias=BT[:, h, i, j:j + 1]),
                             reads=[ps, BT], writes=[PT])
                    if j == i:
                        fw.issue(fw.dve, lambda: nc.vector.tensor_tensor(out=PM[:], in0=PT[:], in1=cm[:], op=ALU.mult), reads=[PT, cm], writes=[PM])
                        P = PM
                    else:
                        P = PT
                else:
                    fw.issue(fw.act, lambda: nc.scalar.activation(out=PT[:], in_=src, func=AF.Exp), reads=[ps], writes=[PT])
                    eng = fw.dve if (m % 3 != 2) else fw.pool
                    fw.issue(eng, lambda: eng.obj.tensor_tensor(out=PM[:], in0=PT[:], in1=mk[:, i - j, :], op=ALU.mult), reads=[PT, mk], writes=[PM])
                    P = PM
                todo.append((il, i, P))
            for il, i, P in todo:
                po = pO[il]
                fw.issue(fw.pe, lambda: nc.tensor.matmul(po[:, 0:65], lhsT=P[:], rhs=Vaug[:, j, hh, :], start=(j == 0), stop=(j == i)),
                         reads=[P, Vaug], writes=[po])
            if j == 4 * I + 3:
                epilogue(u, I, hh)

        LA = 2
        for n in range(min(LA, nsteps)):
            stage_s(n)
        for n in range(nsteps):
            if n + LA < nsteps:
                stage_s(n + LA)
            stage_e(n)
        fw.barrier()
        fw.es = None


def emit_mix_mlstm(fw, tabs, prm, cst, merged):
    nc = fw.nc
    NCH = S // 128
    with ExitStack() as es:
        fw.es = es
        c3 = fw.sb("ml_c3", [128, 3, 128], F32)
        ones = fw.sb("ml_ones", [128, 128], F32)
        cm = fw.sb("ml_cm", [128, 128], F32)
        idb = make_identity(fw, "ml_idb", BF16)
        fw.dma(fw.sp, c3, c3[:], cst["c3"], cst["c3"][:])
        fw.issue(fw.pool, lambda: nc.gpsimd.memset(ones[:], 1.0), writes=[ones])
        qkT = fw.sb("ml_qkT", [128, 4, S], BF16)
        Vm = fw.sb("ml_V", [128, NCH, 2, 257], BF16)
        fw.issue(fw.pool, lambda: nc.gpsimd.memset(Vm[:, :, :, 256:257], 1.0), writes=[Vm])
        SG = fw.sb("ml_SG", [128, NCH, 512], BF16)
        ig = fw.sb("ml_ig", [128, NCH, 2], F32)
        lf = fw.sb("ml_lf", [128, NCH, 2], F32)
        bm = fw.sb("ml_bm", [128, NCH, 2], F32)
        gb = fw.sb("ml_gb", [128, 4], F32)
        ibv = prm["mlstm_ib"].h.rearrange("(o j) -> o j", o=1)
        fbv = prm["mlstm_fb"].h.rearrange("(o j) -> o j", o=1)
        fw.dma(fw.sp, gb, gb[:, 0:2], prm["mlstm_ib"], ibv[:, :].partition_broadcast(128))
        fw.dma(fw.sp, gb, gb[:, 2:4], prm["mlstm_fb"], fbv[:, :].partition_broadcast(128))
        gts = fw.sb("ml_gts", [128, NCH, 8], F32)
        for i in range(NCH):
            gather_rows(fw, gts, gts[:, i, :], tabs["gate"], tabs["idx"], 16 + i)
        fw.issue(fw.dve, lambda: nc.vector.tensor_tensor(out=ig[:], in0=gts[:, :, 4:6], in1=gb[:, 0:2].unsqueeze(1).to_broadcast([128, NCH, 2]), op=ALU.add),
                 reads=[gts, gb], writes=[ig])
        fw.issue(fw.dve, lambda: nc.vector.tensor_tensor(out=lf[:], in0=gts[:, :, 6:8], in1=gb[:, 2:4].unsqueeze(1).to_broadcast([128, NCH, 2]), op=ALU.add),
                 reads=[gts, gb], writes=[lf])
        fw.issue(fw.act, lambda: nc.scalar.activation(out=lf[:], in_=lf[:], func=AF.Exp, scale=-1.0), reads=[lf], writes=[lf])
        fw.issue(fw.act, lambda: nc.scalar.activation(out=lf[:], in_=lf[:], func=AF.Ln, bias=1.0), reads=[lf], writes=[lf])
        fw.issue(fw.dve, lambda: nc.vector.tensor_scalar_mul(out=lf[:], in0=lf[:], scalar1=-1.0), reads=[lf], writes=[lf])
        with ExitStack() as es1:
            fw.es = es1
            pb = fw.ps("ml_pb", [128, 512], F32)
            fw.issue(fw.pe, lambda: nc.tensor.matmul(pb[:, 0:2 * NCH], lhsT=c3[:, 0, :], rhs=lf[:].rearrange("p i c -> p (i c)"),
                                                     start=True, stop=True), reads=[c3, lf], writes=[pb])
            fw.issue(fw.dve, lambda: nc.vector.tensor_tensor(out=bm[:].rearrange("p i c -> p (i c)"), in0=ig[:].rearrange("p i c -> p (i c)"),
                                                             in1=pb[:, 0:2 * NCH], op=ALU.subtract), reads=[ig, pb], writes=[bm])
            cw = fw.sb("ml_cw", [128, 4, 4], F32)
            fw.dma(fw.sp, cw, cw[:], prm["convT"], prm["convT"].h.rearrange("(m p) j -> p m j", p=128))
            raws = [fw.sb(f"ml_raw{i}", [128, S], BF16) for i in range(2)]
            acc = fw.sb("ml_acc", [128, S], F32)
            for m in range(4):
                raw = raws[m % 2]
                for r_ in range(2):
                    gather_rows(fw, raw, raw[:, r_ * 1024:(r_ + 1) * 1024], tabs["feat"], tabs["idxf"], r_ * 4 + m)
                fw.issue(fw.dve, lambda: nc.vector.tensor_scalar_mul(out=acc[:], in0=raw[:], scalar1=cw[:, m, 3:4]), reads=[raw, cw], writes=[acc])
                for sh in (1, 2, 3):
                    fw.issue(fw.dve, lambda: nc.vector.scalar_tensor_tensor(out=acc[:, sh:], in0=raw[:, 0:S - sh], scalar=cw[:, m, 3 - sh:4 - sh],
                                                                            in1=acc[:, sh:], op0=ALU.mult, op1=ALU.add),
                             reads=[raw, cw, acc], writes=[acc])
                if m < 2:
                    fw.issue(fw.act, lambda: nc.scalar.activation(out=qkT[:, m, :], in_=acc[:], func=AF.Silu), reads=[acc], writes=[qkT])
                else:
                    fw.issue(fw.act, lambda: nc.scalar.activation(out=acc[:], in_=acc[:], func=AF.Silu), reads=[acc], writes=[acc])
                    fw.issue(fw.dve, lambda: nc.vector.tensor_scalar_mul(out=qkT[:, m, :], in0=acc[:], scalar1=128 ** -0.5), reads=[acc], writes=[qkT])
            vfs = [fw.sb(f"ml_vf{i}", [128, 2568], BF16) for i in range(4)]
            for c in range(NCH):
                vf = vfs[c % 4]
                if c == 0:
                    for c2_ in range(3):
                        gather_rows(fw, vfs[c2_ % 4], vfs[c2_ % 4][:, :], tabs["tok"], tabs["idx"], c2_)
                if c + 3 < NCH:
                    gather_rows(fw, vfs[(c + 3) % 4], vfs[(c + 3) % 4][:, :], tabs["tok"], tabs["idx"], c + 3)
                eng = fw.pool if c % 2 == 0 else fw.dve
                fw.issue(eng, lambda: eng.obj.tensor_copy(out=Vm[:, c, :, 0:256], in_=vf[:, 1536:2048].rearrange("p (h d) -> p h d", d=256)), reads=[vf], writes=[Vm])
                fw.issue(fw.act, lambda: nc.scalar.activation(out=SG[:, c, :], in_=vf[:, 2048:2560], func=AF.Sigmoid), reads=[vf], writes=[SG])
            fw.barrier()
        fw.es = es
        fw.issue(fw.dve, lambda: nc.vector.tensor_copy(out=cm[:], in_=c3[:, 0, :]), reads=[c3], writes=[cm])
        pB = fw.ps("ml_pB", [128, 4, 128], F32)
        pSs = fw.ps("ml_pS", [128, 4, 128], F32)
        pND = [fw.ps(f"ml_pND{i}", [128, 512], F32) for i in range(2)]
        pDC = [fw.ps(f"ml_pDC{i}", [128, 512], F32) for i in range(2)]
        pTR = fw.ps("ml_pTR", [128, 8, 128], BF16)
        CT = [fw.sb(f"ml_CT{h}", [128, 257], F32) for h in range(2)]
        CTb = [fw.sb(f"ml_CTb{h}", [128, 257], BF16) for h in range(2)]
        NR = 6
        TL = [fw.sb(f"ml_TL{i}", [128, 128], F32) for i in range(NR)]
        ET = [fw.sb(f"ml_ET{i}", [128, 128], F32) for i in range(NR)]
        EB = [fw.sb(f"ml_EB{i}", [128, 128], F32) for i in range(NR)]
        EM = [fw.sb(f"ml_EM{i}", [128, 128], F32) for i in range(NR)]
        WT = [fw.sb(f"ml_WT{i}", [128, 128], BF16) for i in range(NR)]
        QS = [fw.sb(f"ml_QS{i}", [128, 128], BF16) for i in range(NR)]
        KW = [fw.sb(f"ml_KW{i}", [128, 128], BF16) for i in range(NR)]
        hm = [fw.sb(f"ml_hm{i}", [128, 256], F32) for i in range(NR)]
        ho = [fw.sb(f"ml_ho{i}", [128, 256], BF16) for i in range(NR)]
        sq = fw.sb("ml_sq", [128, 256], F32)
        st = [fw.sb(f"ml_st{i}", [128, 4], F32) for i in range(NR)]
        def part_p(n):
            c, h = n // 2, n % 2
            cs_ = slice(c * 128, (c + 1) * 128)
            r = n % NR; sl = n % 4
            tl = TL[r]; et = ET[r]; eb = EB[r]; em = EM[r]; wt = WT[r]; qs = QS[r]; kw = KW[r]
            fw.issue(fw.dve, lambda: nc.vector.tensor_scalar_mul(out=tl[:], in0=c3[:, 0, :], scalar1=lf[:, c, h:h + 1]), reads=[c3, lf], writes=[tl])
            fw.issue(fw.pe, lambda: nc.tensor.matmul(pB[:, sl, :], lhsT=ones[:], rhs=tl[:], start=True, stop=True), reads=[ones, tl], writes=[pB])
            fw.issue(fw.act, lambda: nc.scalar.activation(out=et[:], in_=pB[:, sl, :], func=AF.Exp, bias=bm[:, c, h:h + 1]), reads=[pB, bm], writes=[et])
            fw.issue(fw.act, lambda: nc.scalar.activation(out=eb[:], in_=pB[:, sl, :], func=AF.Exp), reads=[pB], writes=[eb])
            fw.issue(fw.pe, lambda: nc.tensor.matmul(pSs[:, sl, :], lhsT=qkT[:, 2 + h, cs_], rhs=qkT[:, h, cs_], start=True, stop=True),
                     reads=[qkT], writes=[pSs])
            fw.issue(fw.pool, lambda: nc.gpsimd.tensor_tensor(out=em[:], in0=et[:], in1=cm[:], op=ALU.mult), reads=[et, cm], writes=[em])
            fw.issue(fw.dve, lambda: nc.vector.tensor_tensor(out=wt[:], in0=pSs[:, sl, :], in1=em[:], op=ALU.mult), reads=[pSs, em], writes=[wt])
            fw.issue(fw.pool, lambda: nc.gpsimd.tensor_tensor(out=qs[:], in0=qkT[:, h, cs_], in1=eb[:], op=ALU.mult), reads=[qkT, eb], writes=[qs])
            if c < NCH - 1:
                fw.issue(fw.pe, lambda: nc.tensor.transpose(pTR[:, sl, :], qkT[:, 2 + h, cs_], idb[:]), reads=[qkT, idb], writes=[pTR])
                fw.issue(fw.dve, lambda: nc.vector.tensor_scalar_mul(out=kw[:], in0=pTR[:, sl, :], scalar1=et[:, 127:128]), reads=[pTR, et], writes=[kw])

        def part_q(n):
            c, h = n // 2, n % 2
            cs_ = slice(c * 128, (c + 1) * 128)
            r = n % NR
            eb = EB[r]; wt = WT[r]; qs = QS[r]; kw = KW[r]
            pnd = pND[n % 2]
            fw.issue(fw.pe, lambda: nc.tensor.matmul(pnd[:, 0:257], lhsT=wt[:], rhs=Vm[:, c, h, :], start=True, stop=(c == 0)),
                     reads=[wt, Vm], writes=[pnd])
            if c > 0:
                fw.issue(fw.pe, lambda: nc.tensor.matmul(pnd[:, 0:257], lhsT=qs[:], rhs=CTb[h][:], start=False, stop=True),
                         reads=[qs, CTb[h]], writes=[pnd])
            if c < NCH - 1:
                pdc = pDC[n % 2]
                fw.issue(fw.pe, lambda: nc.tensor.matmul(pdc[:, 0:257], lhsT=kw[:], rhs=Vm[:, c, h, :], start=True, stop=True),
                         reads=[kw, Vm], writes=[pdc])
                if c == 0:
                    fw.issue(fw.dve, lambda: nc.vector.tensor_copy(out=CT[h][:], in_=pdc[:, 0:257]), reads=[pdc], writes=[CT[h]])
                else:
                    fw.issue(fw.dve, lambda: nc.vector.scalar_tensor_tensor(out=CT[h][:], in0=CT[h][:], scalar=eb[:, 127:128], in1=pdc[:, 0:257],
                                                                            op0=ALU.mult, op1=ALU.add), reads=[CT[h], eb, pdc], writes=[CT[h]])
                fw.issue(fw.act, lambda: nc.scalar.copy(out=CTb[h][:], in_=CT[h][:]), reads=[CT[h]], writes=[CTb[h]])
            s_ = st[r]
            fw.issue(fw.dve, lambda: nc.vector.tensor_scalar(out=s_[:, 3:4], in0=pnd[:, 256:257], scalar1=-1.0, scalar2=1.0, op0=ALU.mult, op1=ALU.max),
                     reads=[pnd], writes=[s_])
            fw.issue(fw.dve, lambda: nc.vector.tensor_tensor(out=s_[:, 0:1], in0=pnd[:, 256:257], in1=s_[:, 3:4], op=ALU.max), reads=[pnd, s_], writes=[s_])
            fw.issue(fw.dve, lambda: nc.vector.reciprocal(out=s_[:, 0:1], in_=s_[:, 0:1]), reads=[s_], writes=[s_])
            fw.issue(fw.dve, lambda: nc.vector.tensor_scalar_mul(out=hm[r][:], in0=pnd[:, 0:256], scalar1=s_[:, 0:1]), reads=[pnd, s_], writes=[hm[r]])
            fw.issue(fw.dve, lambda: nc.vector.scalar_tensor_tensor(out=sq[:], in0=hm[r][:], scalar=1.0, in1=hm[r][:], op0=ALU.mult, op1=ALU.mult,
                                                                    accum_out=s_[:, 1:2]), reads=[hm[r]], writes=[sq, s_])
            rstd_from_ss(fw, s_, 1.0 / 256, c0=1, c1=2, w=1)
            fw.issue(fw.dve, lambda: nc.vector.scalar_tensor_tensor(out=ho[r][:], in0=hm[r][:], scalar=s_[:, 2:3], in1=SG[:, c, h * 256:(h + 1) * 256],
                                                                    op0=ALU.mult, op1=ALU.mult),
                     reads=[hm[r], s_, SG], writes=[ho[r]])
            fw.dma(fw.sp, merged, merged[cs_, 512 + h * 256:512 + (h + 1) * 256], ho[r], ho[r][:])

        NS = 2 * NCH
        LA = 3
        for n in range(min(LA, NS)):
            part_p(n)
        for n in range(NS):
            if n + LA < NS:
                part_p(n + LA)
            part_q(n)
        fw.barrier()
        fw.es = None


def emit_wout(fw, xsrc, mrg, idxm, modd, out_norm, w_out, x1d):
    nc = fw.nc
    NTL = NT // 128
    with ExitStack() as es:
        fw.es = es
        idb = make_identity(fw, "wo_idb", BF16)
        mT = fw.sb("wo_mT", [128, 16, NT], BF16)
        on_b = fw.sb("wo_on", [128, D], F32)
        g1_b = fw.sb("wo_g1", [128, D], F32)
        mts = [fw.sb(f"wo_mt{i}", [128, D], BF16) for i in range(8)]
        mbs = [fw.sb(f"wo_mb{i}", [128, D], BF16) for i in range(2)]
        trs = [fw.ps(f"wo_tr{i}", [128, 1024], BF16) for i in range(2)]
        mv = modd.h.rearrange("(o j) -> o j", o=1)
        ov = out_norm.h.rearrange("(o j) -> o j", o=1)
        fw.dma(fw.sp, on_b, on_b[:], out_norm, ov[:, :].partition_broadcast(128))
        fw.dma(fw.sp, g1_b, g1_b[:], modd, mv[:, 2 * D:3 * D].partition_broadcast(128))
        for i in range(NTL):
            for r_ in range(2):
                gather_rows(fw, mts[i % 8], mts[i % 8][:, r_ * 1024:(r_ + 1) * 1024], mrg, idxm, r_ * (NT // 128) + i)
        for i in range(NTL):
            mt = mts[i % 8]; mb = mbs[i % 2]
            fw.issue(fw.dve, lambda: nc.vector.tensor_tensor(out=mb[:], in0=mt[:], in1=on_b[:], op=ALU.mult), reads=[mt, on_b], writes=[mb])
            for half in range(2):
                tr = trs[half]
                for kk in range(8):
                    k = half * 8 + kk
                    fw.issue(fw.pe, lambda: nc.tensor.transpose(tr[:, kk * 128:(kk + 1) * 128], mb[:, k * 128:(k + 1) * 128], idb[:]),
                             reads=[mb, idb], writes=[tr])
                dst = mT[:, half * 8:(half + 1) * 8, i * 128:(i + 1) * 128]
                src = tr[:].rearrange("p (k t) -> p k t", k=8)
                if half == 0:
                    fw.issue(fw.act, lambda: nc.scalar.copy(out=dst, in_=src), reads=[tr], writes=[mT])
                else:
                    fw.issue(fw.pool if False else fw.dve, lambda: nc.vector.tensor_copy(out=dst, in_=src), reads=[tr], writes=[mT])
        wts = [fw.sb(f"wo_w{i}", [128, 16, 512], BF16) for i in range(2)]
        xcs = [fw.sb(f"wo_xc{i}", [128, 512], F32) for i in range(4)]
        t1s = [fw.sb(f"wo_t1{i}", [128, 512], F32) for i in range(4)]
        pss = [fw.ps(f"wo_p{i}", [128, 512], F32) for i in range(4)]
        wv = w_out.h.rearrange("(k p) j -> p k j", p=128)
        cnt = 0
        for n in range(4):
            w = wts[n % 2]
            ns = slice(n * 512, (n + 1) * 512)
            fw.dma(fw.gq, w, w[:], w_out, wv[:, :, ns])
            for i in range(NTL):
                p = pss[cnt % 4]; xc = xcs[cnt % 4]; t1 = t1s[cnt % 4]
                rows = slice(i * 128, (i + 1) * 128)
                fw.dma(fw.sp, xc, xc[:], xsrc, xsrc[rows, ns])
                for k in range(16):
                    fw.issue(fw.pe, lambda: nc.tensor.matmul(p[:], lhsT=mT[:, k, rows], rhs=w[:, k, :], start=(k == 0), stop=(k == 15)),
                             reads=[mT, w], writes=[p])
                fw.issue(fw.dve, lambda: nc.vector.tensor_tensor(out=t1[:], in0=p[:], in1=g1_b[:, ns], op=ALU.mult), reads=[p, g1_b], writes=[t1])
                fw.issue(fw.pool, lambda: nc.gpsimd.tensor_tensor(out=t1[:], in0=t1[:], in1=xc[:], op=ALU.add), reads=[t1, xc], writes=[t1])
                fw.dma(fw.sp, x1d, x1d[rows, ns], t1, t1[:])
                cnt += 1
        fw.barrier()
        fw.es = None


def emit_peer_route(fw, yT, wq, k1, k2, cst, idxT, gT, dense=None):
    nc = fw.nc
    NTL = NT // 128
    with ExitStack() as es:
        fw.es = es
        idf = make_identity(fw, "pr_idf", F32)
        iota = fw.sb("pr_iota", [128, 128], F32)
        fw.dma(fw.sp, iota, iota[:], cst["iota"], cst["iota"][:])
        V16 = fw.sb("pr_V16", [128, NTL, 16, 16], F32)
        I16 = fw.sb("pr_I16", [128, NTL, 16, 16], U32)
        kT = fw.sb("pr_kT", [128, 2, 128], F32)
        kst = fw.sb("pr_kst", [128, 2, 128], F32)
        with ExitStack() as es1:
            fw.es = es1
            pk = fw.ps("pr_pk", [128, 2, 128], F32)
            for sd, kk in enumerate((k1, k2)):
                fw.dma(fw.sp, kst, kst[:, sd, :], kk, kk[:])
                fw.issue(fw.pe, lambda: nc.tensor.transpose(pk[:, sd, :], kst[:, sd, :], idf[:]), reads=[kst, idf], writes=[pk])
            fw.issue(fw.act, lambda: nc.scalar.copy(out=kT[:], in_=pk[:]), reads=[pk], writes=[kT])
            wts = [fw.sb(f"pr_w{i}", [128, 16, 512], BF16) for i in range(2)]
            qps = [fw.sb(f"pr_qp{i}", [128, 512], F32) for i in range(2)]
            scs = [fw.sb(f"pr_sc{i}", [128, 128], F32) for i in range(4)]
            wks = [fw.sb(f"pr_wk{i}", [128, 128], F32) for i in range(4)]
            pqs = [fw.ps(f"pr_pq{i}", [128, 512], F32) for i in range(2)]
            pscs = [fw.ps(f"pr_ps{i}", [128, 4, 128], F32) for i in range(2)]
            wv = wq.h.rearrange("(k p) j -> p k j", p=128)
            cnt = 0
            c2 = 0
            for n in range(4):
                w = wts[n % 2]
                fw.dma(fw.gq, w, w[:], wq, wv[:, :, n * 512:(n + 1) * 512])
                for mm in range(4):
                    m = n * 4 + mm
                    side = m % 2
                    for th in range(NT // 512):
                        pq = pqs[cnt % 2]; qp = qps[cnt % 2]
                        for k in range(16):
                            fw.issue(fw.pe, lambda: nc.tensor.matmul(pq[:], lhsT=w[:, k, mm * 128:(mm + 1) * 128], rhs=yT[:, k, th * 512:(th + 1) * 512],
                                                                     start=(k == 0), stop=(k == 15)), reads=[w, yT], writes=[pq])
                        fw.issue(fw.act, lambda: nc.scalar.copy(out=qp[:], in_=pq[:]), reads=[pq], writes=[qp])
                        cnt += 1
                        for tt in range(4):
                            tl_i = th * 4 + tt
                            psc = pscs[(c2 // 4) % 2]; sl = c2 % 4
                            sc = scs[c2 % 4]; wk = wks[c2 % 4]
                            fw.issue(fw.pe, lambda: nc.tensor.matmul(psc[:, sl, :], lhsT=qp[:, tt * 128:(tt + 1) * 128], rhs=kT[:, side, :],
                                                                     start=True, stop=True), reads=[qp, kT], writes=[psc])
                            fw.issue(fw.act, lambda: nc.scalar.copy(out=sc[:], in_=psc[:, sl, :]), reads=[psc], writes=[sc])
                            v = V16[:, tl_i, m, :]; ix = I16[:, tl_i, m, :]
                            fw.issue(fw.dve, lambda: nc.vector.max(out=v[:, 0:8], in_=sc[:]), reads=[sc], writes=[V16])
                            fw.issue(fw.dve, lambda: nc.vector.max_index(out=ix[:, 0:8], in_max=v[:, 0:8], in_values=sc[:]), reads=[sc, V16], writes=[I16])
                            fw.issue(fw.dve, lambda: nc.vector.match_replace(out=wk[:], in_to_replace=v[:, 0:8], in_values=sc[:], imm_value=-1e30),
                                     reads=[sc, V16], writes=[wk])
                            fw.issue(fw.dve, lambda: nc.vector.max(out=v[:, 8:16], in_=wk[:]), reads=[wk], writes=[V16])
                            fw.issue(fw.dve, lambda: nc.vector.max_index(out=ix[:, 8:16], in_max=v[:, 8:16], in_values=wk[:]), reads=[wk, V16], writes=[I16])
                            c2 += 1
            fw.barrier()
        fw.es = es
        cand = fw.sb("pr_cand", [128, 8, 256], F32)
        cwk = fw.sb("pr_cwk", [128, 256], F32)
        tv = fw.sb("pr_tv", [128, 8, 16], F32)
        tc_ = fw.sb("pr_tc", [128, 8, 16], U32)
        hi = fw.sb("pr_hi", [128, 128], I32)
        lo = fw.sb("pr_lo", [128, 128], I32)
        hif = fw.sb("pr_hif", [128, 128], F32)
        lof = fw.sb("pr_lof", [128, 128], F32)
        i1f = fw.sb("pr_i1f", [128, 8, 16], F32)
        i2f = fw.sb("pr_i2f", [128, 8, 16], F32)
        oh = fw.sb("pr_oh", [128, 128, 16], F32)
        e1 = fw.sb("pr_e1", [128, 128], F32)
        e2 = fw.sb("pr_e2", [128, 128], F32)
        eid = fw.sb("pr_eid", [128, 128], F32)
        gg = fw.sb("pr_gg", [128, 8, 16], F32)
        sm = fw.sb("pr_sm", [128, 24], F32)
        ptr = fw.ps("pr_ptr", [128, 2, 128], F32)
        io16 = iota[:, 0:16].unsqueeze(1).to_broadcast([128, 128, 16])
        for i in range(NTL):
            vv = V16[:, i, :, :].rearrange("p (h s) k -> p h s k", s=2)
            ii = I16[:, i, :, :].rearrange("p (h s) k -> p h s k", s=2)
            fw.issue(fw.dve, lambda: nc.vector.tensor_tensor(out=cand[:].rearrange("p h (a b) -> p h a b", b=16),
                                                             in0=vv[:, :, 0, :].unsqueeze(3).to_broadcast([128, 8, 16, 16]),
                                                             in1=vv[:, :, 1, :].unsqueeze(2).to_broadcast([128, 8, 16, 16]), op=ALU.add),
                     reads=[V16], writes=[cand])
            fw.issue(fw.dve, lambda: nc.vector.tensor_copy(out=i1f[:], in_=ii[:, :, 0, :]), reads=[I16], writes=[i1f])
            fw.issue(fw.dve, lambda: nc.vector.tensor_copy(out=i2f[:], in_=ii[:, :, 1, :]), reads=[I16], writes=[i2f])
            for h in range(8):
                fw.issue(fw.dve, lambda: nc.vector.max(out=tv[:, h, 0:8], in_=cand[:, h, :]), reads=[cand], writes=[tv])
                fw.issue(fw.dve, lambda: nc.vector.max_index(out=tc_[:, h, 0:8], in_max=tv[:, h, 0:8], in_values=cand[:, h, :]), reads=[cand, tv], writes=[tc_])
                fw.issue(fw.dve, lambda: nc.vector.match_replace(out=cwk[:], in_to_replace=tv[:, h, 0:8], in_values=cand[:, h, :], imm_value=-1e30),
                         reads=[cand, tv], writes=[cwk])
                fw.issue(fw.dve, lambda: nc.vector.max(out=tv[:, h, 8:16], in_=cwk[:]), reads=[cwk], writes=[tv])
                fw.issue(fw.dve, lambda: nc.vector.max_index(out=tc_[:, h, 8:16], in_max=tv[:, h, 8:16], in_values=cwk[:]), reads=[cwk, tv], writes=[tc_])
            fw.issue(fw.dve, lambda: nc.vector.tensor_scalar_mul(out=sm[:, 0:8], in0=tv[:, :, 0], scalar1=-1.0), reads=[tv], writes=[sm])
            for h in range(8):
                fw.issue(fw.act, lambda: nc.scalar.activation(out=gg[:, h, :], in_=tv[:, h, :], func=AF.Exp, bias=sm[:, h:h + 1], accum_out=sm[:, 8 + h:9 + h]),
                         reads=[tv, sm], writes=[gg, sm])
            fw.issue(fw.dve, lambda: nc.vector.reciprocal(out=sm[:, 16:24], in_=sm[:, 8:16]), reads=[sm], writes=[sm])
            fw.issue(fw.dve, lambda: nc.vector.tensor_tensor(out=gg[:], in0=gg[:], in1=sm[:, 16:24].unsqueeze(2).to_broadcast([128, 8, 16]), op=ALU.mult),
                     reads=[gg, sm], writes=[gg])
            tci = tc_[:].rearrange("p h k -> p (h k)").bitcast(I32)
            fw.issue(fw.dve, lambda: nc.vector.tensor_single_scalar(out=hi[:], in_=tci, scalar=4, op=ALU.arith_shift_right), reads=[tc_], writes=[hi])
            fw.issue(fw.dve, lambda: nc.vector.tensor_single_scalar(out=lo[:], in_=tci, scalar=15, op=ALU.bitwise_and), reads=[tc_], writes=[lo])
            fw.issue(fw.dve, lambda: nc.vector.tensor_copy(out=hif[:], in_=hi[:]), reads=[hi], writes=[hif])
            fw.issue(fw.dve, lambda: nc.vector.tensor_copy(out=lof[:], in_=lo[:]), reads=[lo], writes=[lof])
            for (xf, tab, eo) in ((hif, i1f, e1), (lof, i2f, e2)):
                fw.issue(fw.dve, lambda: nc.vector.tensor_tensor(out=oh[:], in0=xf[:].unsqueeze(2).to_broadcast([128, 128, 16]), in1=io16, op=ALU.is_equal),
                         reads=[xf, iota], writes=[oh])
                fw.issue(fw.pool, lambda: nc.gpsimd.tensor_tensor(out=oh[:].rearrange("p (h k) i -> p h k i", h=8),
                                                                  in0=oh[:].rearrange("p (h k) i -> p h k i", h=8),
                                                                  in1=tab[:].unsqueeze(2).to_broadcast([128, 8, 16, 16]), op=ALU.mult),
                         reads=[oh, tab], writes=[oh])
                fw.issue(fw.dve, lambda: nc.vector.tensor_reduce(out=eo[:], in_=oh[:], axis=AX.X, op=ALU.add), reads=[oh], writes=[eo])
            fw.issue(fw.dve, lambda: nc.vector.scalar_tensor_tensor(out=eid[:], in0=e1[:], scalar=128.0, in1=e2[:], op0=ALU.mult, op1=ALU.add),
                     reads=[e1, e2], writes=[eid])
            if dense is not None:
                e1T, e2T = dense
                fw.issue(fw.pe, lambda: nc.tensor.transpose(ptr[:, 0, :], e1[:], idf[:]), reads=[e1, idf], writes=[ptr])
                fw.issue(fw.pe, lambda: nc.tensor.transpose(ptr[:, 1, :], e2[:], idf[:]), reads=[e2, idf], writes=[ptr])
                fw.issue(fw.dve, lambda: nc.vector.tensor_copy(out=e1T[:, i * 128:(i + 1) * 128], in_=ptr[:, 0, :]), reads=[ptr], writes=[e1T])
                fw.issue(fw.dve, lambda: nc.vector.tensor_copy(out=e2T[:, i * 128:(i + 1) * 128], in_=ptr[:, 1, :]), reads=[ptr], writes=[e2T])
                fw.issue(fw.pe, lambda: nc.tensor.transpose(ptr[:, 1, :], gg[:].rearrange("p h k -> p (h k)"), idf[:]), reads=[gg, idf], writes=[ptr])
                fw.issue(fw.dve, lambda: nc.vector.tensor_copy(out=gT[:, i * 128:(i + 1) * 128], in_=ptr[:, 1, :]), reads=[ptr], writes=[gT])
                continue
            fw.issue(fw.pe, lambda: nc.tensor.transpose(ptr[:, 0, :], eid[:], idf[:]), reads=[eid, idf], writes=[ptr])
            fw.issue(fw.pe, lambda: nc.tensor.transpose(ptr[:, 1, :], gg[:].rearrange("p h k -> p (h k)"), idf[:]), reads=[gg, idf], writes=[ptr])
            fw.issue(fw.dve, lambda: nc.vector.tensor_copy(out=idxT[:, i * 128:(i + 1) * 128], in_=ptr[:, 0, :]), reads=[ptr], writes=[idxT])
            fw.issue(fw.dve, lambda: nc.vector.tensor_copy(out=gT[:, i * 128:(i + 1) * 128], in_=ptr[:, 1, :]), reads=[ptr], writes=[gT])
        fw.barrier()
        fw.es = None


def emit_peer_apply(fw, yd, idxT, gT, u_tab, v_tab, modd, x1d, xout, cst):
    nc = fw.nc
    NTL = NT // 128
    with ExitStack() as es:
        fw.es = es
        iota = fw.sb("pa_iota", [128, 128], F32)
        fw.dma(fw.sp, iota, iota[:], cst["iota"], cst["iota"][:])
        g2_b = fw.sb("pa_g2", [128, D], F32)
        mv = modd.h.rearrange("(o j) -> o j", o=1)
        fw.dma(fw.sp, g2_b, g2_b[:], modd, mv[:, 5 * D:6 * D].partition_broadcast(128))
        NB = 4
        Ub = [fw.sb(f"pa_U{i}", [128, D], BF16) for i in range(NB)]
        Yb = [fw.sb(f"pa_Y{i}", [128, D], BF16) for i in range(NB)]
        junk = fw.sb("pa_junk", [128, D], BF16)
        Wd = [fw.sb(f"pa_Wd{i}", [128, 128], BF16) for i in range(NB)]
        A = [fw.sb(f"pa_A{i}", [128, 128], F32) for i in range(2)]
        W = [fw.sb(f"pa_W{i}", [128, 128], F32) for i in range(2)]
        pO = [fw.ps(f"pa_pO{i}", [128, 512], F32) for i in range(4)]
        xcs = [fw.sb(f"pa_xc{i}", [128, 512], F32) for i in range(2)]
        t1s = [fw.sb(f"pa_t1{i}", [128, 512], F32) for i in range(2)]
        n = 0
        for i in range(NTL):
            a = A[i % 2]; w = W[i % 2]
            for tl in range(128):
                t = i * 128 + tl
                ub = Ub[n % NB]; yb = Yb[n % NB]
                fw.issue(fw.gq, lambda: nc.gpsimd.indirect_dma_start(out=ub[:, :], out_offset=None, in_=u_tab[:, :],
                                                                     in_offset=bass.IndirectOffsetOnAxis(ap=idxT[:, t:t + 1], axis=0)),
                         reads=[u_tab, idxT], writes=[ub])
                fw.dma(fw.sp, yb, yb[:], yd, yd[t:t + 1, :].partition_broadcast(128))
                fw.issue(fw.dve, lambda: nc.vector.scalar_tensor_tensor(out=junk[:], in0=ub[:], scalar=1.0, in1=yb[:], op0=ALU.mult, op1=ALU.mult,
                                                                        accum_out=a[:, tl:tl + 1]), reads=[ub, yb], writes=[junk, a])
                n += 1
            fw.issue(fw.act, lambda: nc.scalar.activation(out=w[:], in_=a[:], func=AF.Gelu), reads=[a], writes=[w])
            fw.issue(fw.dve, lambda: nc.vector.tensor_tensor(out=w[:], in0=w[:], in1=gT[:, i * 128:(i + 1) * 128], op=ALU.mult), reads=[w, gT], writes=[w])
            for tl in range(128):
                t = i * 128 + tl
                vb = Ub[n % NB]; wd = Wd[n % NB]
                fw.issue(fw.gq, lambda: nc.gpsimd.indirect_dma_start(out=vb[:, :], out_offset=None, in_=v_tab[:, :],
                                                                     in_offset=bass.IndirectOffsetOnAxis(ap=idxT[:, t:t + 1], axis=0)),
                         reads=[v_tab, idxT], writes=[vb])
                fw.issue(fw.dve, lambda: nc.vector.tensor_scalar(out=wd[:], in0=iota[:], scalar1=float(tl), scalar2=w[:, tl:tl + 1],
                                                                 op0=ALU.is_equal, op1=ALU.mult), reads=[iota, w], writes=[wd])
                for c in range(4):
                    fw.issue(fw.pe, lambda: nc.tensor.matmul(pO[c][:], lhsT=wd[:], rhs=vb[:, c * 512:(c + 1) * 512], start=(tl == 0), stop=(tl == 127)),
                             reads=[wd, vb], writes=[pO[c]])
                n += 1
            rows = slice(i * 128, (i + 1) * 128)
            for c in range(4):
                xc = xcs[c % 2]; t1 = t1s[c % 2]
                cs_ = slice(c * 512, (c + 1) * 512)
                fw.dma(fw.sp, xc, xc[:], x1d, x1d[rows, cs_])
                fw.issue(fw.dve, lambda: nc.vector.tensor_tensor(out=t1[:], in0=pO[c][:], in1=g2_b[:, cs_], op=ALU.mult), reads=[pO[c], g2_b], writes=[t1])
                fw.issue(fw.pool, lambda: nc.gpsimd.tensor_tensor(out=t1[:], in0=t1[:], in1=xc[:], op=ALU.add), reads=[t1, xc], writes=[t1])
                fw.dma(fw.sp, xout, xout[rows, cs_], t1, t1[:])
        fw.barrier()
        fw.es = None


def emit_peer_G(fw, e1T, e2T, gT, cst, Gd):
    nc = fw.nc
    TB = 256
    with ExitStack() as es:
        fw.es = es
        iota = fw.sb("pg_iota", [128, 128], F32)
        iob = fw.sb("pg_iob", [128, 128], BF16)
        fw.dma(fw.sp, iota, iota[:], cst["iota"], cst["iota"][:])
        fw.issue(fw.dve, lambda: nc.vector.tensor_copy(out=iob[:], in_=iota[:]), reads=[iota], writes=[iob])
        Gs = fw.sb("pg_Gs", [128, 128, TB], BF16)
        NR = 4
        As = [fw.sb(f"pg_A{i}", [128, 4, 128], BF16) for i in range(NR)]
        Bs = [fw.sb(f"pg_B{i}", [128, 4, 128], BF16) for i in range(NR)]
        pG = [fw.ps(f"pg_p{i}", [128, 4, 128], F32) for i in range(2)]
        n = 0
        for tb in range(NT // TB):
            for q in range(TB // 4):
                pg = pG[q % 2]
                t0 = tb * TB + q * 4
                A = As[n % NR]; Bg = Bs[n % NR]
                n += 1
                pgv = pg[:].rearrange("p t e -> p (t e)").rearrange("p (e t) -> p t e", t=4)
                for j in range(4):
                    t = t0 + j
                    fw.issue(fw.dve, lambda: nc.vector.tensor_scalar(out=A[:, j, :], in0=iob[:], scalar1=e1T[:, t:t + 1], scalar2=None, op0=ALU.is_equal),
                             reads=[iob, e1T], writes=[A])
                    fw.issue(fw.dve, lambda: nc.vector.tensor_scalar(out=Bg[:, j, :], in0=iob[:], scalar1=e2T[:, t:t + 1], scalar2=gT[:, t:t + 1],
                                                                     op0=ALU.is_equal, op1=ALU.mult), reads=[iob, e2T, gT], writes=[Bg])
                for j in range(4):
                    fw.issue(fw.pe, lambda: nc.tensor.matmul(pgv[:, j, :], lhsT=Bg[:, j, :], rhs=A[:, j, :], start=True, stop=True), reads=[A, Bg], writes=[pg])
                src = pg[:].rearrange("p t e -> p (t e)").rearrange("p (e t) -> p e t", t=4)
                fw.issue(fw.act, lambda: nc.scalar.copy(out=Gs[:, :, q * 4:q * 4 + 4], in_=src), reads=[pg], writes=[Gs])
            for e0 in range(0, 128, 32):
                fw.dma(fw.sp, Gd, Gd.h[e0:e0 + 32, :, tb * TB:(tb + 1) * TB].rearrange("a b t -> b a t"), Gs, Gs[:, e0:e0 + 32, :])
        fw.barrier()
        fw.es = None


def emit_peer_dense(fw, yT, Gd, u_tab, v_tab, modd, x1d, xout):
    nc = fw.nc
    NTL = NT // 128
    CG = 2
    with ExitStack() as es:
        fw.es = es
        idb = make_identity(fw, "pd_idb", BF16)
        Oacc = [[fw.sb(f"pd_O{tt}_{dc}", [128, 512], F32) for dc in range(4)] for tt in range(NTL)]
        ub = [fw.sb(f"pd_u{i}", [128, CG, D], BF16) for i in range(2)]
        vb = [fw.sb(f"pd_v{i}", [128, CG, D], BF16) for i in range(2)]
        gt = [fw.sb(f"pd_g{i}", [128, CG, NT], BF16) for i in range(2)]
        uT = [fw.sb(f"pd_uT{i}", [128, CG, 16, 128], BF16) for i in range(2)]
        Ga = [fw.sb(f"pd_Ga{i}", [128, NT], BF16) for i in range(2)]
        WT = [fw.sb(f"pd_WT{i}", [128, CG, NT], BF16) for i in range(2)]
        tmp = [fw.sb(f"pd_tmp{i}", [128, 512], F32) for i in range(2)]
        pA = [fw.ps(f"pd_pA{i}", [128, 512], F32) for i in range(2)]
        ptr = [fw.ps(f"pd_ptr{i}", [128, 1024], BF16) for i in range(2)]
        pO = [fw.ps(f"pd_pO{i}", [128, 512], F32) for i in range(4)]
        uv = u_tab.h.rearrange("(c p) d -> p c d", p=128)
        vv = v_tab.h.rearrange("(c p) d -> p c d", p=128)
        NG = 128 // CG
        cnt = dict(na=0, no=0)

        def stage_a(gi):
            b = gi % 2
            c0 = gi * CG
            if gi == 0:
                fw.dma(fw.gq, ub[b], ub[b][:], u_tab, uv[:, c0:c0 + CG, :])
            fw.dma(fw.gq, vb[b], vb[b][:], v_tab, vv[:, c0:c0 + CG, :])
            fw.dma(fw.sp, gt[b], gt[b][:], Gd, Gd.h[c0:c0 + CG, :, :].rearrange("c e t -> e c t"))
            for cc in range(CG):
                for half in range(2):
                    tr = ptr[half]
                    for kk in range(8):
                        k = half * 8 + kk
                        fw.issue(fw.pe, lambda: nc.tensor.transpose(tr[:, kk * 128:(kk + 1) * 128], ub[b][:, cc, k * 128:(k + 1) * 128], idb[:]),
                                 reads=[ub[b], idb], writes=[tr])
                        yield
                    dst = uT[b][:, cc, half * 8:(half + 1) * 8, :]
                    src = tr[:].rearrange("p (k e) -> p k e", k=8)
                    if half == 0:
                        fw.issue(fw.act, lambda: nc.scalar.copy(out=dst, in_=src), reads=[tr], writes=[uT[b]])
                    else:
                        fw.issue(fw.dve, lambda: nc.vector.tensor_copy(out=dst, in_=src), reads=[tr], writes=[uT[b]])
                if cc > 0:
                    yield from a_mm(b, cc - 1)
            if gi + 1 < NG:
                fw.dma(fw.gq, ub[1 - b], ub[1 - b][:], u_tab, uv[:, c0 + CG:c0 + 2 * CG, :])
            yield from a_mm(b, CG - 1)

        def a_mm(b, cc):
            ga = Ga[cc % 2]
            for th in range(NT // 512):
                pa = pA[cnt["na"] % 2]
                cnt["na"] += 1
                for k in range(16):
                    fw.issue(fw.pe, lambda: nc.tensor.matmul(pa[:], lhsT=uT[b][:, cc, k, :], rhs=yT[:, k, th * 512:(th + 1) * 512],
                                                             start=(k == 0), stop=(k == 15)), reads=[uT[b], yT], writes=[pa])
                    yield
                fw.issue(fw.act, lambda: nc.scalar.activation(out=ga[:, th * 512:(th + 1) * 512], in_=pa[:], func=AF.Gelu), reads=[pa], writes=[ga])
            fw.issue(fw.dve, lambda: nc.vector.tensor_tensor(out=WT[b][:, cc, :], in0=ga[:], in1=gt[b][:, cc, :], op=ALU.mult),
                     reads=[ga, gt[b]], writes=[WT[b]])

        def stage_o(gi):
            b = gi % 2
            for tt in range(NTL):
                for dc in range(4):
                    no = cnt["no"]
                    po = pO[no % 4]
                    for cc in range(CG):
                        fw.issue(fw.pe, lambda: nc.tensor.matmul(po[:], lhsT=WT[b][:, cc, tt * 128:(tt + 1) * 128], rhs=vb[b][:, cc, dc * 512:(dc + 1) * 512],
                                                                 start=(cc == 0), stop=(cc == CG - 1)), reads=[WT[b], vb[b]], writes=[po])
                    ot = Oacc[tt][dc]
                    osl = ot[:]
                    if gi == 0:
                        fw.issue(fw.act, lambda: nc.scalar.copy(out=osl, in_=po[:]), reads=[po], writes=[ot])
                    elif no % 4 != 3:
                        fw.issue(fw.dve, lambda: nc.vector.tensor_tensor(out=osl, in0=po[:], in1=osl, op=ALU.add), reads=[po, ot], writes=[ot])
                    else:
                        tm = tmp[(no // 4) % 2]
                        fw.issue(fw.act, lambda: nc.scalar.copy(out=tm[:], in_=po[:]), reads=[po], writes=[tm])
                        fw.issue(fw.pool, lambda: nc.gpsimd.tensor_tensor(out=osl, in0=tm[:], in1=osl, op=ALU.add), reads=[tm, ot], writes=[ot])
                    cnt["no"] += 1
                    yield

        for _ in stage_a(0):
            pass
        for gi in range(NG):
            ga_ = stage_a(gi + 1) if gi + 1 < NG else iter(())
            for _ in stage_o(gi):
                for _k in range(3):
                    next(ga_, None)
            for _ in ga_:
                pass
        g2_b = fw.sb("pd_g2", [128, D], F32)
        mv = modd.h.rearrange("(o j) -> o j", o=1)
        fw.dma(fw.sp, g2_b, g2_b[:], modd, mv[:, 5 * D:6 * D].partition_broadcast(128))
        xcs = [fw.sb(f"pd_xc{i}", [128, D], F32) for i in range(2)]
        for tt in range(NTL):
            xc = xcs[tt % 2]
            rows = slice(tt * 128, (tt + 1) * 128)
            fw.dma(fw.sp, xc, xc[:], x1d, x1d[rows, :])
            for dc in range(4):
                ot = Oacc[tt][dc]
                cs_ = slice(dc * 512, (dc + 1) * 512)
                fw.issue(fw.dve, lambda: nc.vector.tensor_tensor(out=ot[:], in0=ot[:], in1=g2_b[:, cs_], op=ALU.mult), reads=[ot, g2_b], writes=[ot])
                fw.issue(fw.pool, lambda: nc.gpsimd.tensor_tensor(out=xc[:, cs_], in0=xc[:, cs_], in1=ot[:], op=ALU.add), reads=[xc, ot], writes=[xc])
            fw.dma(fw.sp, xout, xout[rows, :], xc, xc[:])
        fw.barrier()
        fw.es = None


def emit_r3(fw, xsrc, mrg, idxm, modd, out_norm, w_out, norm_ffn, wq, k1, k2, u_tab, v_tab, cst, x1d, yd, xout, Gd, dbg=None, skip_apply=False):
    nc = fw.nc
    emit_wout(fw, xsrc, mrg, idxm, modd, out_norm, w_out, x1d)
    with ExitStack() as es:
        fw.es = es
        yT = fw.sb("r3_yT", [128, 16, NT], BF16)
        with ExitStack() as es2:
            fw.es = es2
            idb = make_identity(fw, "r3_idb", BF16)
            emit_norm_mod(fw, x1d, modd, 3 * D, 4 * D, norm_ffn, yT, idb, nt_tiles=NT // 128, ydst=None)
            fw.barrier()
        with ExitStack() as es3:
            fw.es = es3
            e1T = fw.sb("r3_e1T", [128, NT], F32)
            e2T = fw.sb("r3_e2T", [128, NT], F32)
            gT = fw.sb("r3_gT", [128, NT], F32)
            emit_peer_route(fw, yT, wq, k1, k2, cst, None, gT, dense=(e1T, e2T))
            fw.es = es3
            emit_peer_G(fw, e1T, e2T, gT, cst, Gd)
        fw.es = es
        if not skip_apply:
            emit_peer_dense(fw, yT, Gd, u_tab, v_tab, modd, x1d, xout)
        fw.es = None


B_ = 4
NCORES = 8
DEPTH = 2
_PROG = {}


def _host_consts():
    k = np.arange(128)[:, None]
    t = np.arange(128)[None, :]
    tri = (k <= t).astype(np.float32)
    sel = np.zeros((128, 128), np.float32)
    sel[127, :] = 1.0
    idn = np.eye(128, dtype=np.float32)
    c3 = np.ascontiguousarray(np.stack([tri, sel, idn], axis=1))
    dm = np.zeros((128, 16, 128), np.float32)
    for dl in range(16):
        delta = dl * 128 + t - k
        m = ((delta >= 0) & (delta <= 128)).astype(np.float32)
        m += ((delta >= 0) & (delta % 4 == 0) & (delta <= 512)).astype(np.float32)
        m += ((delta >= 0) & (delta % 16 == 0) & (delta <= 2048)).astype(np.float32)
        dm[:, dl, :] = m
    inv = np.power(10000.0, -np.arange(0, 64, 2, dtype=np.float32) / 64).astype(np.float32)
    ang = np.arange(2048, dtype=np.float32)[:, None] * inv[None, :]
    cs = np.concatenate([np.cos(ang), np.sin(ang)], axis=1).astype(np.float32)
    iota = np.ascontiguousarray(np.tile(np.arange(128, dtype=np.float32)[None, :], (128, 1)))
    return dict(c3=c3, dmask=dm, cs=cs, iota=iota)


_LAYER_IN = (("ada_w", [D, 3 * D]), ("ada_b", [3 * D]), ("norm_mix", [D]), ("w_in", [D, 6176]),
             ("fox_qn", [64]), ("fox_kn", [64]), ("dil_qn", [64]), ("dil_kn", [64]), ("fox_fb", [4]),
             ("mlstm_ib", [2]), ("mlstm_fb", [2]), ("convT", [512, 4]), ("out_norm", [D]), ("w_out", [D, D]),
             ("norm_ffn", [D]), ("wq", [D, D]), ("k1", [128, 128]), ("k2", [128, 128]),
             ("u_tab", [16384, D]), ("v_tab", [16384, D]))


_DBG = dict(depth=DEPTH, skip_apply=False, skip_mix=False)


def _build():
    depth = _DBG["depth"]
    nc = bass.Bass("TRN2", target_bir_lowering=False)
    fw = FW(nc)
    x = fw.dram("x", [NT, D], F32, kind="ExternalInput")
    cT = fw.dram("cT", [128, 16], F32, kind="ExternalInput")
    cst = {k: fw.dram(k, shp, F32, kind="ExternalInput") for k, shp in
           (("c3", [128, 3, 128]), ("dmask", [128, 16, 128]), ("cs", [S, 64]), ("iota", [128, 128]))}
    idxd = {k: fw.dram(k, shp, I32, kind="ExternalInput") for k, shp in
            (("idx_tok", [128, 32]), ("idx_feat", [128, 8]), ("idx_mrg", [128, 16]))}
    L = [{nm: fw.dram(f"{nm}_{l}", shp, F32, kind="ExternalInput") for nm, shp in _LAYER_IN} for l in range(DEPTH)]
    out = fw.dram("out", [NT, D], F32, kind="ExternalOutput")
    idx = {k: fw.sb("s_" + k, shp, I32) for k, shp in (("idx_tok", [128, 32]), ("idx_feat", [128, 8]), ("idx_mrg", [128, 16]))}
    for k in idx:
        fw.dma(fw.sp, idx[k], idx[k][:], idxd[k], idxd[k][:])
    xcur = x
    for l in range(depth):
        W = L[l]
        modh = fw.dram(f"modh_{l}", [3 * D], F32)
        modd = fw.dram(f"modd_{l}", [6 * D], F32)
        pj_tok = fw.dram(f"pj_tok_{l}", [NT, 5136], BF16)
        pj_feat = fw.dram(f"pj_feat_{l}", [1024, NT], BF16)
        pj_gate = fw.dram(f"pj_gate_{l}", [NT, 16], F32)
        pjt_all = fw.dram(f"pjt_all_{l}", [2 * NT, 5136], BF16)
        pjf_all = fw.dram(f"pjf_all_{l}", [2048, NT], BF16)
        pjg_all = fw.dram(f"pjg_all_{l}", [2 * NT, 16], F32)
        mg_src = fw.dram(f"mg_src_{l}", [S, 1024], BF16)
        mg_all = fw.dram(f"mg_all_{l}", [2 * S, 1024], BF16)
        x1d = fw.dram(f"x1d_{l}", [NT, D], F32)
        yd = None
        Gd = fw.dram(f"Gd_{l}", [128, 128, NT], BF16)
        xnext = out if l == depth - 1 else fw.dram(f"xmid_{l}", [NT, D], F32)
        emit_r1(fw, xcur, cT, W["ada_w"], W["ada_b"], W["norm_mix"], W["w_in"], modh, modd, pj_tok, pj_feat, pj_gate)
        allgather(fw, pj_tok, pjt_all, 128)
        allgather(fw, pj_feat, pjf_all, 512)
        allgather(fw, pj_gate, pjg_all, NT)
        tabs = dict(tok=alias(pjt_all, [4 * NT, 2568]), gate=alias(pjg_all, [4 * NT, 8]), feat=pjf_all,
                    idx=idx["idx_tok"], idxf=idx["idx_feat"])
        prm = {k: W[k] for k in ("fox_qn", "fox_kn", "dil_qn", "dil_kn", "fox_fb", "mlstm_ib", "mlstm_fb", "convT")}
        emit_mix_attn(fw, tabs, prm, cst, mg_src)
        emit_mix_mlstm(fw, tabs, prm, cst, mg_src)
        allgather(fw, mg_src, mg_all, 1024)
        emit_r3(fw, xcur, mg_all, idx["idx_mrg"], modd, W["out_norm"], W["w_out"], W["norm_ffn"], W["wq"], W["k1"], W["k2"],
                W["u_tab"], W["v_tab"], cst, x1d, yd, xnext, Gd, skip_apply=_DBG["skip_apply"])
        xcur = xnext
    fw.barrier()
    return nc


def _perm_w_in():
    cols = []
    for g in range(2):
        for base, w in ((0, 256), (512, 256), (1536, 256), (2048, 256), (1024, 256), (2560, 256)):
            cols += list(range(base + g * 256, base + g * 256 + w))
        cols += list(range(4096 + g * 512, 4096 + (g + 1) * 512))
        cols += list(range(5120 + g * 512, 5120 + (g + 1) * 512))
        cols += list(range(6144 + g * 4, 6144 + (g + 1) * 4))
        cols += list(range(6152 + g * 2, 6152 + (g + 1) * 2))
        cols += list(range(6156 + g * 2, 6156 + (g + 1) * 2))
    for g in range(2):
        cols += list(range(3072 + g * 256, 3072 + (g + 1) * 256))
        cols += list(range(3584 + g * 256, 3584 + (g + 1) * 256))
    for g in range(2):
        cols += list(range(6144 + g * 4, 6144 + (g + 1) * 4))
        cols += list(range(6152 + g * 2, 6152 + (g + 1) * 2))
        cols += list(range(6156 + g * 2, 6156 + (g + 1) * 2))
    assert len(cols) == 6176
    return np.asarray(cols)


def _perm_merged():
    rows = []
    for g in range(2):
        rows += list(range(g * 256, (g + 1) * 256))
        rows += list(range(512 + g * 256, 512 + (g + 1) * 256))
        rows += list(range(1024 + g * 512, 1024 + (g + 1) * 512))
    return np.asarray(rows)


def kernel(**inp):
    inp = {k: np.asarray(v) for k, v in inp.items()}
    hc = _host_consts()
    pw = _perm_w_in()
    pm = _perm_merged()
    shared = []
    for l in range(DEPTH):
        shared.append({
            f"norm_mix_{l}": np.ascontiguousarray(inp["norm_mix"][l]), f"w_in_{l}": np.ascontiguousarray(inp["w_in"][l][:, pw]),
            f"fox_qn_{l}": np.ascontiguousarray(inp["fox_qn"][l]), f"fox_kn_{l}": np.ascontiguousarray(inp["fox_kn"][l]),
            f"dil_qn_{l}": np.ascontiguousarray(inp["dil_qn"][l]), f"dil_kn_{l}": np.ascontiguousarray(inp["dil_kn"][l]),
            f"out_norm_{l}": np.ascontiguousarray(inp["out_norm"][l][pm]), f"w_out_{l}": np.ascontiguousarray(inp["w_out"][l][pm, :]),
            f"norm_ffn_{l}": np.ascontiguousarray(inp["norm_ffn"][l]), f"wq_{l}": np.ascontiguousarray(inp["peer_wq"][l]),
            f"k1_{l}": np.ascontiguousarray(inp["peer_k1"][l]), f"k2_{l}": np.ascontiguousarray(inp["peer_k2"][l]),
            f"u_tab_{l}": np.ascontiguousarray(inp["peer_u"][l]), f"v_tab_{l}": np.ascontiguousarray(inp["peer_v"][l]),
        })
    p = np.arange(128, dtype=np.int64)[:, None]
    adah = [[(np.ascontiguousarray(inp["ada_w"][l][:, g * 3 * D:(g + 1) * 3 * D]), np.ascontiguousarray(inp["ada_b"][l][g * 3 * D:(g + 1) * 3 * D]))
             for g in range(2)] for l in range(DEPTH)]
    in_maps = []
    for c in range(NCORES):
        b, g = c // 2, c % 2
        m = {"x": np.ascontiguousarray(inp["x"][b, g * NT:(g + 1) * NT]),
             "cT": np.ascontiguousarray(inp["c"][b].reshape(16, 128).T),
             "c3": hc["c3"], "dmask": hc["dmask"], "cs": hc["cs"], "iota": hc["iota"]}
        i16 = np.arange(16, dtype=np.int64)[None, :]
        Tk = i16 * 128 + p
        r_, t_ = Tk // NT, Tk % NT
        it = ((((t_ // 128) * 2 + r_) * 128 + t_ % 128) * 2 + g)
        ig_ = (r_ * NT + t_) * 2 + g
        m["idx_tok"] = np.ascontiguousarray(np.concatenate([it, ig_], axis=1).astype(np.int32))
        rm = np.arange(8, dtype=np.int64)[None, :]
        m["idx_feat"] = np.ascontiguousarray(((g * 2 + rm // 4) * 512 + (rm % 4) * 128 + p).astype(np.int32))
        Tm = g * NT + (i16 % 8) * 128 + p
        m["idx_mrg"] = np.ascontiguousarray((((Tm // 1024) * 2 + i16 // 8) * 1024 + Tm % 1024).astype(np.int32))
        for l in range(DEPTH):
            m.update(shared[l])
            convT = inp["mlstm_conv"][l].T
            m[f"convT_{l}"] = np.ascontiguousarray(np.concatenate([convT[g * 256:(g + 1) * 256], convT[512 + g * 256:512 + (g + 1) * 256]], axis=0))
            m[f"fox_fb_{l}"] = np.ascontiguousarray(inp["fox_fb"][l][g * 4:(g + 1) * 4])
            m[f"ada_w_{l}"] = adah[l][g][0]
            m[f"ada_b_{l}"] = adah[l][g][1]
            m[f"mlstm_ib_{l}"] = np.ascontiguousarray(inp["mlstm_ib"][l][g * 2:(g + 1) * 2])
            m[f"mlstm_fb_{l}"] = np.ascontiguousarray(inp["mlstm_fb"][l][g * 2:(g + 1) * 2])
        in_maps.append(m)
    if "nc" not in _PROG:
        _PROG["nc"] = _build()
    res = run_bass_kernel_spmd(_PROG["nc"], in_maps, core_ids=list(range(NCORES)))
    out = np.empty((B_, S, D), np.float32)
    for c in range(NCORES):
        out[c // 2, (c % 2) * NT:(c % 2 + 1) * NT] = res.results[c]["out"]
    return out
```
